# Optimizing a Trainium2 kernel written in Bass

```python
import jax, jax.numpy as jnp
from jax import lax
import numpy as np


D_MODEL = 1024
BATCH = 32
SEQ = 2048
DEPTH = 1

ATTN_WIDTH = D_MODEL // 2
HEAD_DIM = 64
N_HEADS = ATTN_WIDTH // HEAD_DIM
POOL_WIDTH = D_MODEL - ATTN_WIDTH
POOL_WINDOWS = (2, 4, 8, 16)
N_POOL_GROUPS = len(POOL_WINDOWS)
POOL_GROUP = POOL_WIDTH // N_POOL_GROUPS
IN_WIDTH = 3 * ATTN_WIDTH + POOL_WIDTH
MOBA_BLOCK = 256
MOBA_TOPK = 3
QUERY_CHUNK = 16
ROPE_THETA = 10000.0
D_FF = 4 * D_MODEL
NORM_EPS = 1e-6

kernel_name = 'hymba_moba_pool_sqrelu_adaln'


def rmsnorm(x, g):
    x32 = x.astype(jnp.float32)
    y = x32 * lax.rsqrt(jnp.mean(x32 * x32, axis=-1, keepdims=True) + NORM_EPS)
    return (y * g.astype(jnp.float32)).astype(x.dtype)


def rope(x, pos):
    half = HEAD_DIM // 2
    inv_freq = 1.0 / (ROPE_THETA ** (jnp.arange(half, dtype=jnp.float32) * (2.0 / HEAD_DIM)))
    ang = pos.astype(jnp.float32)[:, None] * inv_freq[None, :]
    cos = jnp.cos(ang)[None, :, None, :]
    sin = jnp.sin(ang)[None, :, None, :]
    x32 = x.astype(jnp.float32)
    x1, x2 = x32[..., :half], x32[..., half:]
    out = jnp.concatenate([x1 * cos - x2 * sin, x2 * cos + x1 * sin], axis=-1)
    return out.astype(x.dtype)


def moba_attention(q, k, v):
    B, S = q.shape[0], q.shape[1]
    nb = -(-S // MOBA_BLOCK)
    s_pad = nb * MOBA_BLOCK
    k_eff = max(1, min(MOBA_TOPK, nb - 1))
    scale = HEAD_DIM ** -0.5
    qh = q.transpose(0, 2, 1, 3)
    pad = ((0, 0), (0, 0), (0, s_pad - S), (0, 0))
    kb = jnp.pad(k.transpose(0, 2, 1, 3), pad).reshape(B, N_HEADS, nb, MOBA_BLOCK, HEAD_DIM)
    vb = jnp.pad(v.transpose(0, 2, 1, 3), pad).reshape(B, N_HEADS, nb, MOBA_BLOCK, HEAD_DIM)
    k_mean = jnp.mean(kb.astype(jnp.float32), axis=3)
    n_chunks = S // QUERY_CHUNK
    q_chunks = qh.reshape(B, N_HEADS, n_chunks, QUERY_CHUNK, HEAD_DIM).transpose(2, 0, 1, 3, 4)
    bi = jnp.arange(B)[:, None, None, None]
    hi = jnp.arange(N_HEADS)[None, :, None, None]
    key_in_block = jnp.arange(MOBA_BLOCK)
    block_ids = jnp.arange(nb)

    def one_chunk(args):
        q_c, ci = args
        start = ci * QUERY_CHUNK
        blk = start // MOBA_BLOCK
        t = start + jnp.arange(QUERY_CHUNK)
        gate = jnp.einsum('bhqd,bhnd->bhqn', q_c.astype(jnp.float32), k_mean)
        gate = jnp.where(block_ids < blk, gate, -jnp.inf)
        _, idx = lax.top_k(gate, k_eff)
        valid = idx < blk
        k_sel = kb[bi, hi, idx]
        v_sel = vb[bi, hi, idx]
        s_sel = jnp.einsum('bhqd,bhqkpd->bhqkp', q_c, k_sel).astype(jnp.float32) * scale
        s_sel = jnp.where(valid[..., None], s_sel, -jnp.inf)
        s_sel = s_sel.reshape(B, N_HEADS, QUERY_CHUNK, k_eff * MOBA_BLOCK)
        k_own = lax.dynamic_index_in_dim(kb, blk, axis=2, keepdims=False)
        v_own = lax.dynamic_index_in_dim(vb, blk, axis=2, keepdims=False)
        s_own = jnp.einsum('bhqd,bhpd->bhqp', q_c, k_own).astype(jnp.float32) * scale
        causal = (blk * MOBA_BLOCK + key_in_block)[None, :] <= t[:, None]
        s_own = jnp.where(causal, s_own, -jnp.inf)
        p = jax.nn.softmax(jnp.concatenate([s_own, s_sel], axis=-1), axis=-1).astype(v.dtype)
        p_own = p[..., :MOBA_BLOCK]
        p_sel = p[..., MOBA_BLOCK:].reshape(B, N_HEADS, QUERY_CHUNK, k_eff, MOBA_BLOCK)
        return (jnp.einsum('bhqp,bhpd->bhqd', p_own, v_own)
                + jnp.einsum('bhqkp,bhqkpd->bhqd', p_sel, v_sel))

    out = lax.map(one_chunk, (q_chunks, jnp.arange(n_chunks)))
    return out.transpose(1, 0, 3, 2, 4).reshape(B, S, ATTN_WIDTH)


def pool_mixer(u, w_pool, pool_scale):
    B, S = u.shape[0], u.shape[1]
    ug = u.reshape(B, S, N_POOL_GROUPS, POOL_GROUP).astype(jnp.float32)
    cs = jnp.cumsum(ug, axis=1)
    pos = jnp.arange(S)
    diffs = []
    for g, w in enumerate(POOL_WINDOWS):
        c_g = cs[:, :, g]
        window_sum = c_g - jnp.pad(c_g, ((0, 0), (w, 0), (0, 0)))[:, :S]
        count = jnp.minimum(pos + 1, w).astype(jnp.float32)[None, :, None]
        diffs.append(window_sum / count - ug[:, :, g])
    d = jnp.stack(diffs, axis=2).astype(u.dtype)
    y = jnp.einsum('bsgc,gce->bsge', d, w_pool).reshape(B, S, POOL_WIDTH)
    return y * pool_scale


def setup_inputs(seed: int = 0) -> dict:
    key = jax.random.key(seed)
    ks = jax.random.split(key, 16)
    f32 = jnp.float32
    D = D_MODEL
    def nrm(k, shape, s):
        return jax.random.normal(k, shape, f32) * s
    return {
        'x': nrm(ks[0], (BATCH, SEQ, D), 1.0),
        'c': nrm(ks[1], (BATCH, D), 1.0),
        'w_ada': nrm(ks[2], (DEPTH, D, 6 * D), D ** -0.5),
        'b_ada': nrm(ks[3], (DEPTH, 6 * D), 0.02),
        'g_mix_pre': 1.0 + nrm(ks[4], (DEPTH, D), 0.02),
        'g_mix_post': 1.0 + nrm(ks[5], (DEPTH, D), 0.02),
        'w_in': nrm(ks[6], (DEPTH, D, IN_WIDTH), D ** -0.5),
        'w_pool': nrm(ks[7], (DEPTH, N_POOL_GROUPS, POOL_GROUP, POOL_GROUP), POOL_GROUP ** -0.5),
        'pool_scale': 1.0 + nrm(ks[8], (DEPTH, POOL_WIDTH), 0.02),
        'w_out': nrm(ks[9], (DEPTH, ATTN_WIDTH + POOL_WIDTH, D), (ATTN_WIDTH + POOL_WIDTH) ** -0.5),
        'g_mlp_pre': 1.0 + nrm(ks[10], (DEPTH, D), 0.02),
        'g_mlp_post': 1.0 + nrm(ks[11], (DEPTH, D), 0.02),
        'w_up': nrm(ks[12], (DEPTH, D, D_FF), D ** -0.5),
        'w_down': nrm(ks[13], (DEPTH, D_FF, D), D_FF ** -0.5),
    }


def reference(x, c, w_ada, b_ada, g_mix_pre, g_mix_post, w_in, w_pool, pool_scale, w_out,
              g_mlp_pre, g_mlp_post, w_up, w_down):
    B, S = x.shape[0], x.shape[1]
    pos = jnp.arange(S, dtype=jnp.int32)
    c_act = jax.nn.silu(c)
    for l in range(DEPTH):
        mod = c_act @ w_ada[l] + b_ada[l]
        sh1, sc1, ga1, sh2, sc2, ga2 = [m[:, None, :] for m in jnp.split(mod, 6, axis=-1)]
        h = rmsnorm(x, g_mix_pre[l]) * (1.0 + sc1) + sh1
        proj = h @ w_in[l]
        q = proj[..., :ATTN_WIDTH].reshape(B, S, N_HEADS, HEAD_DIM)
        k = proj[..., ATTN_WIDTH:2 * ATTN_WIDTH].reshape(B, S, N_HEADS, HEAD_DIM)
        v = proj[..., 2 * ATTN_WIDTH:3 * ATTN_WIDTH].reshape(B, S, N_HEADS, HEAD_DIM)
        u = proj[..., 3 * ATTN_WIDTH:]
        attn_out = moba_attention(rope(q, pos), rope(k, pos), v)
        pool_out = pool_mixer(u, w_pool[l], pool_scale[l])
        y = jnp.concatenate([attn_out, pool_out], axis=-1) @ w_out[l]
        x = x + ga1 * rmsnorm(y, g_mix_post[l])
        h = rmsnorm(x, g_mlp_pre[l]) * (1.0 + sc2) + sh2
        y = jnp.square(jax.nn.relu(h @ w_up[l])) @ w_down[l]
        x = x + ga2 * rmsnorm(y, g_mlp_post[l])
    return x
```

```python
import numpy as np
from contextlib import ExitStack
import concourse.bass as bass
import concourse.mybir as mybir
from concourse.bass_utils import run_bass_kernel_spmd

F32 = mybir.dt.float32
BF16 = mybir.dt.bfloat16
ALU = mybir.AluOpType
AF = mybir.ActivationFunctionType
AX = mybir.AxisListType

NCORES = 8
NSEQ = 4
S = 2048
D = 1024
T = 512
NCH = S // T
NPIECE = 22
RING = 4
XSLOTS = 8
EPS = 1e-6
NEG = -30000.0
PAIR_EXP = False


class Op:
    __slots__ = ("eng", "fn", "deps", "inc", "count", "dma", "dma_val", "name")


class Sched:
    ENGS = ("pe", "act", "dve", "pool", "sp")

    def __init__(self):
        self.ops = {e: [] for e in self.ENGS}
        self.lastw = {}
        self.readers = {}
        self.dma_cnt = {}

    def add(self, eng, fn, reads=(), writes=(), dma=None, name=""):
        op = Op()
        op.eng, op.fn, op.inc, op.count, op.dma, op.dma_val, op.name = eng, fn, False, 0, dma, 0, name
        deps = []
        seen = set()

        def push(o):
            if o is not None and id(o) not in seen:
                seen.add(id(o))
                deps.append(o)

        for r in reads:
            push(self.lastw.get(r))
        for w in writes:
            push(self.lastw.get(w))
            for o in self.readers.get(w, {}).values():
                push(o)
        op.deps = [d for d in deps if not (d.eng == "pe" and eng == "pe")]
        for d in op.deps:
            if d.dma is None:
                d.inc = True
        for r in reads:
            self.readers.setdefault(r, {})[eng] = op
        for w in writes:
            self.lastw[w] = op
            self.readers[w] = {}
        if dma is not None:
            self.dma_cnt[dma] = self.dma_cnt.get(dma, 0) + 1
            op.dma_val = 16 * self.dma_cnt[dma]
        self.ops[eng].append(op)
        return op

    def finalize(self):
        for e in self.ENGS:
            c = 0
            for op in self.ops[e]:
                if op.inc:
                    c += 1
                    op.count = c

    def emit(self, e, eng, eng_sems, dma_sems, final_waits=()):
        waited = {}
        for op in self.ops[e]:
            for d in op.deps:
                if d.dma is not None:
                    key, val, sem = ("d", d.dma), d.dma_val, dma_sems[d.dma]
                else:
                    key, val, sem = ("e", d.eng), d.count, eng_sems[d.eng]
                if waited.get(key, 0) < val:
                    eng.wait_ge(sem, val)
                    waited[key] = val
            ins = op.fn(eng)
            if op.dma is not None:
                ins.then_inc(dma_sems[op.dma], 16)
            elif op.inc:
                ins.then_inc(eng_sems[e], 1)
        for (sem, val) in final_waits:
            eng.wait_ge(sem, val)


def _alias(key):
    k0 = key[0]
    if k0 == "hid":
        m = key[1]
        if m < 8:
            return [("Q", m)] + [("Qb", t) for t in range(4)] + [("Qb", t, x) for t in range(4) for x in range(2)]
        if m < 12:
            return [("cat", m - 8, 0), ("cat", m - 8, 1)]
        if m < 16:
            return [("cat", m - 8)]
        if m < 20:
            return [("dT", m - 16)]
        out = []
        if m <= 28:
            out += [("ue", g) for g in range(4)]
        if m >= 28:
            out += [("pbuf", 0), ("pbuf", 1)]
        return out
    if k0 == "Q":
        return [("hid", key[1])]
    if k0 == "Qb":
        if len(key) == 3:
            return [("hid", h) for h in range(4 * key[2], 4 * key[2] + 4)]
        return [("hid", h) for h in range(8)]
    if k0 == "cat":
        return [("hid", 8 + key[1])]
    if k0 == "dT":
        return [("hid", 16 + key[1])]
    if k0 == "ue":
        return [("hid", m) for m in range(20, 29)]
    if k0 == "pbuf":
        return [("hid", m) for m in range(28, 32)]
    if k0 == "stg":
        return [("hid", m) for m in range(16 * key[1], 16 * key[1] + 16)]
    return []


def _expand(keys):
    out = []
    seen = set()
    for k in keys:
        for kk in [k] + _alias(k):
            if kk not in seen:
                seen.add(kk)
                out.append(kk)
    return out


class _Stop(Exception):
    pass


def build(nc, nseq=NSEQ, nchunks=None, dumps=(), stop=99):
    total_chunks = nseq * NCH if nchunks is None else nchunks
    ntok = nseq * S
    es = ExitStack()
    sch = Sched()

    def add(eng, fn, reads=(), writes=(), dma=None, name=""):
        return sch.add(eng, fn, _expand(list(reads)), _expand(list(writes)), dma=dma, name=name)

    def dram(name, shape, dt, kind):
        return nc.dram_tensor(name, list(shape), dt, kind=kind).ap()

    x_d = dram("x", [ntok, D], F32, "ExternalInput")
    out_d = dram("out", [ntok, D], F32, "ExternalOutput")
    cT_d = dram("cT", [128, 8, NSEQ], F32, "ExternalInput")
    wada_d = dram("wada", [12, 128, 4096], F32, "ExternalInput")
    bada_d = dram("bada", [128, 48], F32, "ExternalInput")
    gvec_d = dram("gvec", [128, 4, 8], F32, "ExternalInput")
    pscale_d = dram("pscale", [128, 4], F32, "ExternalInput")
    wpool_d = dram("wpool", [128, 4, 128], F32, "ExternalInput")
    W_d = dram("W", [NPIECE, 128, 4096], F32, "ExternalInput")
    cst_d = dram("cst", [128, 6, 128], F32, "ExternalInput")
    rope_d = dram("rope", [64, NCH, 2, T], F32, "ExternalInput")
    kinit_d = dram("kinit", [128, S], F32, "ExternalInput")
    invc_d = dram("invc", [128, 4, 16], F32, "ExternalInput")
    wbf_d = dram("wbf", [NPIECE, 128, 4096], BF16, "Internal")
    dump_d = {}
    for (nm, shape, dt) in dumps:
        dump_d[nm] = dram("dbg_" + nm, shape, dt, "ExternalOutput")

    def sb(name, shape, dt):
        return es.enter_context(nc.sbuf_tensor(name, list(shape), dt))

    xs = [sb(f"xs{i}", [128, D], F32) for i in range(XSLOTS)]
    ring = [sb(f"ring{i}", [128, 8, 512], BF16) for i in range(RING)]
    Kc = sb("Kc", [128, 8, S], BF16)
    Vc = sb("Vc", [128, 16, 4, 160], BF16)
    xn = [sb(f"xn{i}", [128, D], BF16) for i in range(4)]
    hT = sb("hT", [128, 8, T], BF16)
    big = sb("big", [128, 33 * 512], BF16)
    bigv = big[:]
    hid = bigv[:, 0:32 * 512].rearrange("p (m t) -> p m t", m=32)
    Q = bigv[:, 0:4096].rearrange("p (h t) -> p h t", h=8)
    cat = bigv[:, 4096:8192].rearrange("p (h t) -> p h t", h=8)
    dT = bigv[:, 8192:10240].rearrange("p (g t) -> p g t", g=4)
    ue = bigv[:, 10240:14464].bitcast(F32).rearrange("p (g t) -> p g t", g=4)
    pbuf = [bigv[:, 14464:15520].bitcast(F32), bigv[:, 15520:16576].bitcast(F32)]
    stgf = [bigv[:, 0:8192].bitcast(F32), bigv[:, 8192:16384].bitcast(F32)]
    PT = [sb(f"PT{i}", [128, T], BF16) for i in range(4)]
    G = [sb(f"G{i}", [128, D], F32) for i in range(2)]
    ropeb = sb("ropeb", [128, 2, T], F32)
    biasT = [sb(f"biasT{i}", [128, 8, 72], BF16) for i in range(4)]
    junk = sb("junk", [128, D], BF16)
    qraw = [sb(f"qraw{i}", [128, T], BF16) for i in range(2)]
    tf = [sb(f"tf{i}", [128, T], F32) for i in range(4)]
    bcs = sb("bcs", [128, T], F32)
    rr = sb("rr", [128, T], BF16)
    gm = sb("gm", [128, 8, 8], F32)
    cmpb = bcs[:].rearrange("p (a b) -> p a b", b=8)
    rank = sb("rank", [128, 8, 8], F32)
    kmean = sb("kmean", [128, 8, 8], BF16)
    ksum = sb("ksum", [128, 8, 2], F32)
    identf_t = sb("identf", [128, 128], F32)
    cstb = sb("cstb", [128, 6, 128], BF16)
    wpb = sb("wpb", [128, 4, 128], BF16)
    cTs = sb("cTs", [128, 8, NSEQ], F32)
    cact = sb("cact", [128, 8, NSEQ], F32)
    bada = sb("badas", [128, 48], F32)
    gvec = sb("gvecs", [128, 4, 8], F32)
    pscale = sb("pscales", [128, 4], F32)
    invc = sb("invcs", [128, 4, 16], F32)
    modT = sb("modT", [128, 48, NSEQ], F32)
    A1 = sb("A1", [128, 8, NSEQ], F32)
    A2 = sb("A2", [128, 8, NSEQ], F32)
    GA = [sb(f"GA{i}", [128, 8, NSEQ], F32) for i in range(2)]
    gcol = [sb("gcol0", [128, 128], F32)] * 2
    stat = sb("stat", [128, 64], F32)
    tmpf = sb("tmpf", [128, 16], F32)
    epsb = sb("epsb", [128, 1], F32)
    halo = sb("halo", [128, 4, 16], F32)

    psall_t = es.enter_context(nc.psum_tensor("psall", [128, 8 * 512], F32))
    psall = psall_t[:]
    ps = [psall[:, i * 512:(i + 1) * 512] for i in range(8)]

    identb = cstb[:, 0, :]
    rsw = cstb[0:64, 1, 0:64]
    tri = cstb[:, 2, :]
    trineg = cstb[:, 4, :]
    identf = identf_t[:]
    cstf = stgf[0][:, 0:768].rearrange("p (a b) -> p a b", a=6)
    wpf = stgf[0][:, 1024:1536].rearrange("p (a b) -> p a b", a=4)

    bank_state = {"next": 0, "held": set(), "pref": []}

    def alloc_bank():
        while bank_state["pref"]:
            b = bank_state["pref"].pop(0)
            if b not in bank_state["held"]:
                return b
        for _ in range(16):
            b = bank_state["next"]
            bank_state["next"] = (b + 1) % 8
            if b not in bank_state["held"]:
                return b
        raise RuntimeError("no free psum bank")

    def hold(b):
        bank_state["held"].add(b)

    def release(b):
        bank_state["held"].discard(b)

    stat_i = [0]

    def new_stat(n=1):
        i = stat_i[0]
        if i + n > 64:
            i = 0
        stat_i[0] = i + n
        return i

    dma_keys = []

    def dma(out, in_, reads, writes, key):
        if key not in dma_keys:
            dma_keys.append(key)
        return add("sp", lambda e: e.dma_start(out=out, in_=in_), reads=reads, writes=writes, dma=key)

    def dump(nm, src_ap, reads):
        if nm in dump_d:
            dma(dump_d[nm], src_ap, reads, [("dump", nm)], "dump_" + nm)

    def mm_group(out, pairs):
        def f(e):
            ins = None
            n = len(pairs)
            for i, (l, r) in enumerate(pairs):
                ins = e.matmul(out=out, lhsT=l, rhs=r, start=(i == 0), stop=(i == n - 1))
            return ins
        return f

    HT_ALL = [("hT", j) for j in range(8)]
    QB_ALL = [("Qb", t) for t in range(4)] + [("Qb", t, x) for t in range(4) for x in range(2)]
    CAT_ALL = [("cat", i, x) for i in range(4) for x in range(2)] + [("cat", 4 + g) for g in range(4)]

    dma(cstf, cst_d, [], [("stg", 0)], "stg0")
    dma(cTs[:], cT_d, [], [("cT",)], "c1")
    dma(bada[:], bada_d, [], [("bada",)], "c2")
    dma(gvec[:], gvec_d, [], [("gvec",)], "c3")
    dma(pscale[:], pscale_d, [], [("pscale",)], "c4")
    dma(invc[:], invc_d, [], [("invc",)], "c5")
    dma(wpf, wpool_d, [("stg", 0)], [("stg", 0)], "stg0")
    add("dve", lambda e: e.tensor_copy(out=cstb[:], in_=cstf), [("stg", 0)], [("cstb",)])
    add("dve", lambda e: e.tensor_copy(out=wpb[:], in_=wpf), [("stg", 0)], [("wpb",)])
    add("dve", lambda e: e.tensor_copy(out=identf, in_=cstf[:, 0, :]), [("stg", 0)], [("cstf",)])
    add("act", lambda e: e.activation(out=cact[:], in_=cTs[:], func=AF.Silu), [("cT",)], [("cact",)])
    add("pool", lambda e: e.memset(epsb[:], EPS), [], [("epsb",)])

    def conv_quarter(pi, q, rs, rv):
        sl = 4 + q
        if f"x{sl}" not in dma_keys:
            dma_keys.append(f"x{sl}")
        dma(xs[sl][:], W_d[pi][:, q * 1024:(q + 1) * 1024], [], [("x", sl)], f"x{sl}")
        if q % 2 == 0:
            add("dve", lambda e: e.tensor_copy(out=rv[:, q * 1024:(q + 1) * 1024], in_=xs[sl][:]), [("x", sl)], [("ringh", rs, q)])
        else:
            add("act", lambda e: e.copy(out=rv[:, q * 1024:(q + 1) * 1024], in_=xs[sl][:]), [("x", sl)], [("ringh", rs, q)])

    def conv_piece(pi):
        rs = pi % RING
        rv = ring[rs][:].rearrange("p k c -> p (k c)")
        for q in range(4):
            conv_quarter(pi, q, rs, rv)
        dma(wbf_d[pi], rv, [("ringh", rs, q) for q in range(4)], [("wbf", pi), ("ring", rs)], f"ring{rs}")

    modbank = alloc_bank()
    hold(modbank)

    def mod_piece(pi):
        sv = stgf[pi % 2]
        dma(sv, wada_d[pi], [], [("stg", pi % 2)], f"stg{pi % 2}")
        svv = sv.rearrange("p (k c) -> p k c", k=8)

        def f(e):
            ins = None
            for fc in range(4):
                col = (pi * 4 + fc) * NSEQ
                for kk in range(8):
                    ins = e.matmul(out=ps[modbank][:, col:col + NSEQ], lhsT=svv[:, kk, fc * 128:(fc + 1) * 128],
                                   rhs=cact[:, kk, :], start=(kk == 0), stop=(kk == 7))
            return ins
        add("pe", f, [("stg", pi % 2), ("cact",)], [("ps", modbank)])
    ci_ = 0
    for pi in range(12):
        mod_piece(pi)
        for _ in range(2):
            if ci_ < NPIECE:
                conv_piece(ci_)
                ci_ += 1
    add("dve", lambda e: e.tensor_tensor(out=modT[:], in0=ps[modbank][:, 0:48 * NSEQ].rearrange("p (f b) -> p f b", b=NSEQ),
                                        in1=bada[:].unsqueeze(2).broadcast_to([128, 48, NSEQ]), op=ALU.add),
        [("ps", modbank), ("bada",)], [("modT",)])
    release(modbank)

    def gb(w):
        return gvec[:, w, :].unsqueeze(2).broadcast_to([128, 8, NSEQ])
    add("dve", lambda e: e.scalar_tensor_tensor(out=A1[:], in0=modT[:, 8:16, :], scalar=1.0, in1=gb(0), op0=ALU.add, op1=ALU.mult),
        [("modT",), ("gvec",)], [("A1",)])
    add("dve", lambda e: e.scalar_tensor_tensor(out=A2[:], in0=modT[:, 32:40, :], scalar=1.0, in1=gb(2), op0=ALU.add, op1=ALU.mult),
        [("modT",), ("gvec",)], [("A2",)])
    add("dve", lambda e: e.tensor_tensor(out=GA[0][:], in0=modT[:, 16:24, :], in1=gb(1), op=ALU.mult), [("modT",), ("gvec",)], [("GA", 0)])
    add("dve", lambda e: e.tensor_tensor(out=GA[1][:], in0=modT[:, 40:48, :], in1=gb(3), op=ALU.mult), [("modT",), ("gvec",)], [("GA", 1)])

    kin = stgf[0][:, 0:S]
    dma(kin, kinit_d, [], [("stg", 0)], "stg0")

    def kinit_head(h):
        if h % 2 == 0:
            add("dve", lambda e: e.tensor_copy(out=Kc[:, h, :], in_=kin), [("stg", 0)], [("K", h)])
        else:
            add("act", lambda e: e.copy(out=Kc[:, h, :], in_=kin), [("stg", 0)], [("K", h)])
    for h in range(8):
        kinit_head(h)
    add("pool", lambda e: e.memset(Vc[:], 0.0), [], [("Vinit",)])
    add("pool", lambda e: e.memset(Vc[:, :, :, 64:65], 1.0), [("Vinit",)], [("V", kt) for kt in range(16)])
    add("pool", lambda e: e.memset(kmean[:], 0.0), [], [("kmean",)])

    def ring_load(gp):
        if gp >= total_chunks * NPIECE:
            return
        pi = gp % NPIECE
        s_ = gp % RING
        dma(ring[s_][:].rearrange("p k c -> p (k c)"), wbf_d[pi], [("wbf", pi)], [("ring", s_)], f"ring{s_}")

    def xload(tg):
        if tg >= total_chunks * 4:
            return
        sl = tg % XSLOTS
        dma(xs[sl][:], x_d[tg * 128:(tg + 1) * 128, :], [], [("x", sl)], f"x{sl}")

    for gp in range(RING):
        ring_load(gp)
    for tg in range(XSLOTS):
        xload(tg)

    def rstd_from(ss_ap, reads):
        i1 = new_stat()
        i2 = new_stat()
        add("act", lambda e: e.activation(out=stat[:, i1:i1 + 1], in_=ss_ap, func=AF.Ln, scale=1.0 / D, bias=epsb[:, 0:1]),
            list(reads) + [("epsb",)], [("st", i1)])
        add("act", lambda e: e.activation(out=stat[:, i2:i2 + 1], in_=stat[:, i1:i1 + 1], func=AF.Exp, scale=-0.5),
            [("st", i1)], [("st", i2)])
        return i2

    def norm_prep_tile(ci, t):
        sl = (ci * 4 + t) % XSLOTS
        iss = new_stat()
        add("act", lambda e: e.activation(out=junk[:], in_=xs[sl][:], func=AF.Square, accum_out=stat[:, iss:iss + 1]),
            [("x", sl)], [("st", iss), ("junk", 0), ("junk", 1)])
        ir = rstd_from(stat[:, iss:iss + 1], [("st", iss)])
        add("dve", lambda e: e.tensor_scalar(out=xn[t][:], in0=xs[sl][:], scalar1=stat[:, ir:ir + 1], scalar2=None, op0=ALU.mult),
            [("x", sl), ("st", ir)], [("xn", t)])

    def norm_prep(ci):
        for t in range(4):
            norm_prep_tile(ci, t)

    def tp_tile(t, tpbanks):
        def f(e):
            ins = None
            for j in range(8):
                bk = ps[tpbanks[j // 2]][:].bitcast(BF16)
                pos = (j % 2) * 4 + t
                ins = e.transpose(out=bk[:, pos * 128:(pos + 1) * 128], in_=xn[t][:, j * 128:(j + 1) * 128], identity=identb)
            return ins
        add("pe", f, [("xn", t), ("cstb",)], [("ps", tpbanks[jj]) for jj in range(4)])

    def evac_h(j, b, Amat, shbase, tpbanks):
        bk = ps[tpbanks[j // 2]][:].bitcast(BF16)[:, (j % 2) * 512:(j % 2 + 1) * 512]
        sc = Amat[:, j, b:b + 1]
        bi = modT[:, shbase + j, b:b + 1]
        rd = [("ps", tpbanks[j // 2]), ("A1",), ("A2",), ("modT",)]
        if (j // 2) % 2 == 0:
            add("act", lambda e: e.activation(out=hT[:, j, :], in_=bk, func=AF.Identity, scale=sc, bias=bi), rd, [("hT", j)])
        else:
            add("dve", lambda e: e.tensor_scalar(out=hT[:, j, :], in0=bk, scalar1=sc, scalar2=bi, op0=ALU.mult, op1=ALU.add), rd, [("hT", j)])

    def norm_tp(b, Amat, shbase, tpbanks=None):
        if tpbanks is None:
            tpbanks = [alloc_bank() for _ in range(4)]
        for t in range(4):
            tp_tile(t, tpbanks)
        for j in range(8):
            evac_h(j, b, Amat, shbase, tpbanks)

    def epiA(ci, t, banks, Gi):
        i0 = new_stat(2)
        tbs = [(2 * t) % 4, (2 * t + 1) % 4]

        def sq(hf):
            add("act", lambda e: e.activation(out=junk[:, hf * 512:(hf + 1) * 512], in_=ps[banks[hf]][:], func=AF.Square, accum_out=stat[:, i0 + hf:i0 + hf + 1]),
                [("ps", banks[hf])], [("st", i0 + hf), ("junk", hf)])

        def mul(hf):
            tb = tbs[hf]
            add("dve", lambda e: e.tensor_tensor(out=tf[tb][:], in0=ps[banks[hf]][:], in1=G[Gi][:, hf * 512:(hf + 1) * 512], op=ALU.mult),
                [("ps", banks[hf]), ("G", Gi), ("st", i0 + hf)], [("tf", tb)])
        sq(0)
        sq(1)
        mul(0)
        mul(1)
        return (ci, t, i0, tbs)

    def epiB(st, store):
        ci, t, i0, tbs = st
        sl = (ci * 4 + t) % XSLOTS
        isum = new_stat()
        add("dve", lambda e: e.tensor_tensor(out=stat[:, isum:isum + 1], in0=stat[:, i0:i0 + 1], in1=stat[:, i0 + 1:i0 + 2], op=ALU.add),
            [("st", i0), ("st", i0 + 1)], [("st", isum)])
        ir = rstd_from(stat[:, isum:isum + 1], [("st", isum)])

        def stt(hf):
            tb = tbs[hf]
            add("dve", lambda e: e.scalar_tensor_tensor(out=xs[sl][:, hf * 512:(hf + 1) * 512], in0=tf[tb][:], scalar=stat[:, ir:ir + 1],
                                                       in1=xs[sl][:, hf * 512:(hf + 1) * 512], op0=ALU.mult, op1=ALU.add),
                [("tf", tb), ("st", ir), ("x", sl)], [("x", sl)])
        stt(0)
        stt(1)
        if store:
            tg = ci * 4 + t
            dma(out_d[tg * 128:(tg + 1) * 128, :], xs[sl][:], [("x", sl)], [("out", tg)], f"x{sl}")
            xload(tg + XSLOTS)

    def epilogue(ci, t, banks, Gi, store):
        epiB(epiA(ci, t, banks, Gi), store)

    def seq_setup(b):
        def one(w, j, gbanks):
            gi = 0
            add("dve", lambda e: e.tensor_copy(out=gcol[gi][:], in_=GA[w][:, j, b:b + 1].broadcast_to([128, 128])),
                [("GA", w)], [("gcol", gi)])
            add("pe", lambda e: e.matmul(out=ps[gbanks[j // 4]][:, (j % 4) * 128:(j % 4 + 1) * 128], lhsT=gcol[gi][:], rhs=identf, start=True, stop=True),
                [("gcol", gi), ("cstf",)], [("ps", gbanks[j // 4])])

        def ev(w, hf, gbanks):
            add("act", lambda e: e.copy(out=G[w][:, hf * 512:(hf + 1) * 512], in_=ps[gbanks[hf]][:]), [("ps", gbanks[hf])], [("G", w)])
        for w in range(2):
            gbanks = [alloc_bank(), alloc_bank()]
            for j in range(8):
                one(w, j, gbanks)
            for hf in range(2):
                ev(w, hf, gbanks)
        add("pool", lambda e: e.memset(halo[:], 0.0), [], [("halo", g) for g in range(4)])
        add("pool", lambda e: e.memset(gm[:], -1e30), [], [("gm",)])

        def zb(t):
            add("pool", lambda e: e.memset(biasT[t][:], 0.0), [], [("biasT", t)])
        for t in range(4):
            zb(t)

    def proj_step1(i, is_k, rs, cp):
        pbank = alloc_bank()
        add("pe", mm_group(ps[pbank][:], [(ring[rs][:, kk, i * 128:(i + 1) * 128], hT[:, kk, :]) for kk in range(8)]),
            [("ring", rs)] + HT_ALL, [("ps", pbank)])
        qb = i % 2
        add("act", lambda e: e.copy(out=qraw[qb][:], in_=ps[pbank][:]), [("ps", pbank)], [("qraw", qb)])
        return (i, is_k, cp, qb, pbank)

    def proj_step2(st):
        i, is_k, cp, qb, pbank = st
        bR, bS, bT = alloc_bank(), alloc_bank(), alloc_bank()
        add("dve", lambda e: e.tensor_tensor(out=tf[0][0:64, :], in0=ps[pbank][0:64, :], in1=ropeb[0:64, 0, :], op=ALU.mult),
            [("ps", pbank), ("rope",), ("qraw", qb)], [("tf", 0)])

        def f(e):
            e.matmul(out=ps[bR][0:64, :], lhsT=cstb[:, 1, 0:64], rhs=qraw[qb][:], start=True, stop=True)
            e.matmul(out=ps[bS][0:64, :], lhsT=cstb[:, 1, 64:128], rhs=qraw[qb][:], start=True, stop=True)
            return e.matmul(out=ps[bT][0:64, :], lhsT=cstb[:, 5, 0:64], rhs=qraw[qb][:], start=True, stop=True)
        add("pe", f, [("qraw", qb), ("cstb",)], [("ps", bR), ("ps", bS), ("ps", bT)])
        add("dve", lambda e: e.tensor_tensor(out=tf[1][0:64, :], in0=ps[bR][0:64, :], in1=ropeb[0:64, 1, :], op=ALU.mult),
            [("ps", bR), ("rope",)], [("tf", 1)])
        add("dve", lambda e: e.tensor_tensor(out=tf[2][0:64, :], in0=ps[bS][0:64, :], in1=ropeb[0:64, 0, :], op=ALU.mult),
            [("ps", bS), ("rope",)], [("tf", 2)])
        add("dve", lambda e: e.tensor_tensor(out=tf[3][0:64, :], in0=ps[bT][0:64, :], in1=ropeb[0:64, 1, :], op=ALU.mult),
            [("ps", bT), ("rope",)], [("tf", 3)])
        for e_, (ta, tb_) in enumerate(((0, 1), (2, 3))):
            h = 2 * i + e_
            if is_k:
                dst = Kc[0:64, h, cp * T:(cp + 1) * T]
                wr = [("K", h)]
            else:
                dst = Q[0:64, h, :]
                wr = [("Q", h)]
            rope_add(dst, ta, tb_, wr)

    def rope_add(dst, ta, tb_, wr):
        add("pool", lambda e: e.tensor_tensor(out=dst, in0=tf[ta][0:64, :], in1=tf[tb_][0:64, :], op=ALU.add),
            [("tf", ta), ("tf", tb_)], wr)

    def proj_heads(is_k, rs, cp):
        prev = None
        for i in range(4):
            st = proj_step1(i, is_k, rs, cp)
            if prev is not None:
                proj_step2(prev)
            prev = st
        proj_step2(prev)

    def proj_v(t, rs, cp):
        vbank = alloc_bank()
        kt = cp * 4 + t
        add("pe", mm_group(ps[vbank][:], [(hT[:, kk, t * 128:(t + 1) * 128], ring[rs][:, kk, :]) for kk in range(8)]),
            [("ring", rs)] + HT_ALL, [("ps", vbank)])
        vv = ps[vbank][:].rearrange("p (i e d) -> p i e d", i=4, e=2)
        add("act", lambda e: e.copy(out=Vc[:, kt, :, 0:64], in_=vv[:, :, 0, :]), [("ps", vbank)], [("V", kt)])
        add("dve", lambda e: e.tensor_copy(out=Vc[:, kt, :, 96:160], in_=vv[:, :, 1, :]), [("ps", vbank), ("V", kt)], [("V", kt)])

    def proj_u(g, rs, cp):
        ubank = alloc_bank()
        add("pe", mm_group(ps[ubank][:], [(ring[rs][:, kk, g * 128:(g + 1) * 128], hT[:, kk, :]) for kk in range(8)]),
            [("ring", rs)] + HT_ALL, [("ps", ubank)])
        add("act", lambda e: e.copy(out=ue[:, g, 16:528], in_=ps[ubank][:]), [("ps", ubank)], [("ue", g)])

    def pool_chain(g, cp):
        add("pool", lambda e: e.tensor_copy(out=ue[:, g, 0:16], in_=halo[:, g, :]), [("halo", g), ("ue", g)], [("ue", g)])

        def level(lv):
            sh = 1 << lv
            lo = (1 << (lv + 1)) - 1
            dstb = pbuf[lv % 2]
            if lv == 0:
                a0, a1 = ue[:, g, lo:528], ue[:, g, lo - sh:528 - sh]
                rd = [("ue", g)]
            else:
                sb_ = pbuf[(lv - 1) % 2]
                a0, a1 = sb_[:, lo:528], sb_[:, lo - sh:528 - sh]
                rd = [("pbuf", (lv - 1) % 2)]
            add("pool", lambda e: e.tensor_tensor(out=dstb[:, lo:528], in0=a0, in1=a1, op=ALU.add), rd, [("pbuf", lv % 2)])
        for lv in range(g + 1):
            level(lv)
        Lb = pbuf[g % 2]
        w = 1 << (g + 1)
        add("dve", lambda e: e.scalar_tensor_tensor(out=dT[:, g, :], in0=Lb[:, 16:528], scalar=1.0 / w, in1=ue[:, g, 16:528], op0=ALU.mult, op1=ALU.subtract),
            [("pbuf", g % 2), ("ue", g)], [("dT", g)])
        if cp == 0:
            add("dve", lambda e: e.tensor_tensor(out=tmpf[:, 0:15], in0=Lb[:, 16:31], in1=invc[:, g, 0:15], op=ALU.mult),
                [("pbuf", g % 2), ("invc",)], [("tmpf",)])
            add("dve", lambda e: e.tensor_tensor(out=dT[:, g, 0:15], in0=tmpf[:, 0:15], in1=ue[:, g, 16:31], op=ALU.subtract),
                [("tmpf",), ("ue", g), ("dT", g)], [("dT", g)])
        add("pool", lambda e: e.tensor_copy(out=halo[:, g, :], in_=ue[:, g, 512:528]), [("ue", g)], [("halo", g)])

    def gating_part1(cp):
        tiles = [t for t in range(4) if 2 * cp + t // 2 > 0]
        if not tiles:
            return
        gbanks = [alloc_bank(), alloc_bank()]
        for t in tiles:
            gating_scores(t, cp, gbanks[t % 2])

    def gating_scores(t, cp, gbank):
        blk = 2 * cp + t // 2
        c0, c1 = t * 128, (t + 1) * 128

        def f(e):
            ins = None
            for h in range(8):
                ins = e.matmul(out=ps[gbank][:, t * 64 + h * 8:t * 64 + (h + 1) * 8], lhsT=Q[0:64, h, c0:c1], rhs=kmean[0:64, h, :], start=True, stop=True)
            return ins
        add("pe", f, [("Q", h) for h in range(8)] + [("kmean",)], [("ps", gbank)])
        add("dve", lambda e: e.tensor_copy(out=gm[:, :, 0:blk], in_=ps[gbank][:, t * 64:(t + 1) * 64].rearrange("p (h j) -> p h j", h=8)[:, :, 0:blk]),
            [("ps", gbank)], [("gm",)])
        add("dve", lambda e: e.tensor_tensor(out=cmpb.rearrange("p (h j) i -> p h j i", h=8),
                                            in0=gm[:].unsqueeze(2).broadcast_to([128, 8, 8, 8]),
                                            in1=gm[:].unsqueeze(3).broadcast_to([128, 8, 8, 8]), op=ALU.is_gt),
            [("gm",)], [("bcs",)])
        add("dve", lambda e: e.tensor_reduce(out=rank[:].rearrange("p h j -> p (h j)"), in_=cmpb, axis=AX.X, op=ALU.add),
            [("bcs",)], [("rank",)])
        add("dve", lambda e: e.tensor_scalar(out=biasT[t][:, :, 64:64 + blk], in0=rank[:, :, 0:blk], scalar1=2.5, scalar2=NEG, op0=ALU.is_gt, op1=ALU.mult),
            [("rank",)], [("biasT", t)])

    def gating_part2(t, cp):
        blk = 2 * cp + t // 2
        c0, c1 = t * 128, (t + 1) * 128
        if blk == 0:
            add("pool", lambda e: e.memset(Q[64:72, :, c0:c1], 0.0), [], [("Qb", t)])
            return
        qbb = [alloc_bank(), alloc_bank()]

        def f2(e):
            ins = None
            for h in range(8):
                ins = e.matmul(out=ps[qbb[h // 4]][0:72, (h % 4) * 128:(h % 4 + 1) * 128], lhsT=biasT[t][:, h, :], rhs=identb, start=True, stop=True)
            return ins
        add("pe", f2, [("biasT", t), ("cstb",)], [("ps", qbb[0]), ("ps", qbb[1])])
        add("act", lambda e: e.copy(out=Q[64:72, 0:4, c0:c1], in_=ps[qbb[0]][64:72, :].rearrange("p (h c) -> p h c", h=4)),
            [("ps", qbb[0])], [("Qb", t, 0)])
        add("dve", lambda e: e.tensor_copy(out=Q[64:72, 4:8, c0:c1], in_=ps[qbb[1]][64:72, :].rearrange("p (h c) -> p h c", h=4)),
            [("ps", qbb[1])], [("Qb", t, 1)])

    rrf = tf[0]
    ptc = [0]
    sslot = [0]
    accrot = [0]

    def attn_head(i, e_, acc, cp, hook=None):
        h = 2 * i + e_
        units = []
        k0 = 4 * cp
        if PAIR_EXP:
            for u in range(0, 4 * cp, 2):
                units.append([(u, 0, 0, 0, 0), (u + 1, 0, 1, 0, 512)])
            units.append([(k0, 0, 0, 0, 0), (k0 + 1, 128, 1, 0, 512)])
            units.append([(k0 + 2, 256, 0, 0, 0), (k0 + 3, 384, 0, 256, 256)])
        else:
            for u in range(0, 4 * cp):
                units.append([(u, 0, 0, 0, 0)])
            for r in range(4):
                units.append([(k0 + r, 128 * r, 0, 0, 0)])
        nun = len(units)

        def emit_qk(u):
            if PAIR_EXP:
                slot = sslot[0] % 2
                b0 = 2 * slot
                raise RuntimeError("PAIR_EXP unsupported")
            else:
                b0 = sslot[0] % 4
                pb = ptc[0] % 4
                ptv = PT[pb]
            sslot[0] += 1
            ptc[0] += 1
            tl = units[u]
            wbanks = [("ps", b0), ("ps", b0 + 1)] if PAIR_EXP else [("ps", b0)]

            def f(e):
                ins = None
                for (kt, qlo, bi, c0, po) in tl:
                    N = T - qlo
                    diag = kt >= 4 * cp
                    ins = e.matmul(out=ps[b0 + bi][:, c0:c0 + N], lhsT=Kc[0:72, h, kt * 128:(kt + 1) * 128], rhs=Q[0:72, h, qlo:T], start=True, stop=(not diag))
                    if diag:
                        ins = e.matmul(out=ps[b0 + bi][:, c0:c0 + 128], lhsT=identb, rhs=trineg, start=False, stop=True)
                return ins
            add("pe", f, [("K", h), ("Q", h), ("cstb",)] + QB_ALL, wbanks)
            last = tl[-1]
            W = last[4] + (T - last[1])
            add("act", lambda e: e.activation(out=ptv[:, 0:W], in_=psall[:, b0 * 512:b0 * 512 + W], func=AF.Exp, scale=0.125),
                wbanks, [("PT", pb)])
            return (pb, ptv)

        def emit_pv(u, pbv):
            pb, ptv = pbv
            tl = units[u]

            def f(e):
                ins = None
                for ti, (kt, qlo, bi, c0, po) in enumerate(tl):
                    N = T - qlo
                    first = (u == 0 and ti == 0)
                    lastt = (u == nun - 1 and ti == len(tl) - 1)
                    if e_ == 0:
                        o = ps[acc][0:65, qlo:T]
                        l = Vc[:, kt, i, 0:65]
                    else:
                        o = ps[acc][:, qlo:T]
                        l = Vc[:, kt, i, 32:160]
                    ins = e.matmul(out=o, lhsT=l, rhs=ptv[:, po:po + N], start=first, stop=lastt)
                return ins
            add("pe", f, [("V", kt) for (kt, _, _, _, _) in tl] + [("PT", pb)], [("ps", acc)])

        pend = []
        for u in range(nun):
            pend.append((u, emit_qk(u)))
            if len(pend) > (1 if PAIR_EXP else 2):
                emit_pv(*pend.pop(0))
        while pend:
            emit_pv(*pend.pop(0))
        if hook is not None:
            hook()

    def attn_pair(i, cp, prev_finish, last=False):
        a0 = 4 + accrot[0] % 4
        a1 = 4 + (accrot[0] + 1) % 4
        accrot[0] += 2
        attn_head(i, 0, a0, cp, hook=prev_finish)
        attn_head(i, 1, a1, cp)

        def rcp(e, dst, src_):
            with nc.allow_low_precision("softmax normaliser broadcast through a bf16 K=1 matmul"):
                return e.reciprocal(out=dst, in_=src_)
        if last:
            def act_rcp(p0, a, k):
                add("act", lambda e: e.activation(out=rrf[p0:p0 + 1, :], in_=ps[a][p0:p0 + 1, :], func=AF.Ln), [("ps", a)], [("tf", 0)])
                add("act", lambda e: e.activation(out=rr[p0:p0 + 1, :], in_=rrf[p0:p0 + 1, :], func=AF.Exp, scale=-1.0), [("tf", 0)], [("rr", k)])
            act_rcp(64, a0, 0)
            act_rcp(32, a1, 1)
        else:
            add("dve", lambda e: rcp(e, rr[64:65, :], ps[a0][64:65, :]), [("ps", a0)], [("rr", 0)])
            add("dve", lambda e: rcp(e, rr[32:33, :], ps[a1][32:33, :]), [("ps", a1)], [("rr", 1)])

        def finish():
            bcb = 2 * (sslot[0] % 2) if PAIR_EXP else sslot[0] % 4
            sslot[0] += 1

            def fb(e):
                e.matmul(out=ps[bcb][0:64, :], lhsT=cstb[64:65, 3, 0:64], rhs=rr[64:65, :], start=True, stop=True)
                return e.matmul(out=ps[bcb][64:128, :], lhsT=cstb[32:33, 3, 0:64], rhs=rr[32:33, :], start=True, stop=True)
            add("pe", fb, [("rr", 0), ("rr", 1), ("cstb",)], [("ps", bcb)])
            add("dve", lambda e: e.tensor_copy(out=bcs[:], in_=ps[bcb][:]), [("ps", bcb)], [("bcs",)])
            add("dve", lambda e: e.tensor_tensor(out=cat[0:64, i, :], in0=ps[a0][0:64, :], in1=bcs[0:64, :], op=ALU.mult),
                [("ps", a0), ("bcs",)], [("cat", i, 0)])
            add("dve", lambda e: e.tensor_tensor(out=cat[64:128, i, :], in0=ps[a1][64:128, :], in1=bcs[64:128, :], op=ALU.mult),
                [("ps", a1), ("bcs",)], [("cat", i, 1)])
        return finish

    def pool_mm(g):
        pbk = alloc_bank()
        add("pe", lambda e: e.matmul(out=ps[pbk][:], lhsT=wpb[:, g, :], rhs=dT[:, g, :], start=True, stop=True),
            [("wpb",), ("dT", g)], [("ps", pbk)])
        add("dve", lambda e: e.tensor_scalar(out=cat[:, 4 + g, :], in0=ps[pbk][:], scalar1=pscale[:, g:g + 1], scalar2=None, op0=ALU.mult),
            [("ps", pbk), ("pscale",)], [("cat", 4 + g)])

    def wout_tile(ci, t, rs2):
        yb = [alloc_bank(), alloc_bank()]
        for hf in range(2):
            add("pe", mm_group(ps[yb[hf]][:], [(cat[:, kk, t * 128:(t + 1) * 128], ring[rs2[hf]][:, kk, :]) for kk in range(8)]),
                [("ring", rs2[hf])] + CAT_ALL, [("ps", yb[hf])])
        epilogue(ci, t, yb, 0, store=False)

    def wup_chunk(m, rs):
        hb = alloc_bank()
        add("pe", mm_group(ps[hb][:], [(ring[rs][:, kk, (m % 4) * 128:(m % 4 + 1) * 128], hT[:, kk, :]) for kk in range(8)]),
            [("ring", rs)] + HT_ALL, [("ps", hb)])
        rb_ = m % 2
        add("act", lambda e: e.activation(out=tf[rb_][:], in_=ps[hb][:], func=AF.Relu), [("ps", hb)], [("tf", rb_)])
        eng = "dve" if m % 2 == 0 else "pool"
        add(eng, lambda e: e.tensor_tensor(out=hid[:, m, :], in0=tf[rb_][:], in1=tf[rb_][:], op=ALU.mult), [("tf", rb_)], [("hid", m)])

    def wdown_group(hf, kg, t, rs):
        bk = hf * 4 + t

        def f(e):
            ins = None
            for kk in range(8):
                ins = e.matmul(out=ps[bk][:], lhsT=hid[:, kg * 8 + kk, t * 128:(t + 1) * 128], rhs=ring[rs][:, kk, :],
                               start=(kg == 0 and kk == 0), stop=(kg == 3 and kk == 7))
            return ins
        add("pe", f, [("ring", rs)] + [("hid", kg * 8 + kk) for kk in range(8)], [("ps", bk)])

    def do_chunk(ci):
        b = ci // NCH
        cp = ci % NCH
        gp0 = ci * NPIECE

        def rslot(pi):
            return (gp0 + pi) % RING

        if cp == 0:
            seq_setup(b)
        if ci == 0:
            dma(ropeb[0:64, :, :], rope_d[:, cp, :, :], [], [("rope",)], "rope")

        if stop <= 1:
            raise _Stop()
        if ci == 0:
            norm_prep(ci)
            norm_tp(b, A1, 0)
        dump("hT", hT[:], HT_ALL)
        if stop <= 2:
            raise _Stop()
        proj_heads(True, rslot(0), cp)
        ring_load(gp0 + 0 + RING)
        add("dve", lambda e: e.tensor_reduce(out=ksum[0:64, :, :], in_=Kc[0:64, :, cp * T:(cp + 1) * T].rearrange("p h (b t) -> p h b t", b=2), axis=AX.X, op=ALU.add),
            [("K", h) for h in range(8)], [("ksum",)])
        add("dve", lambda e: e.tensor_scalar(out=kmean[0:64, :, 2 * cp:2 * cp + 2], in0=ksum[0:64, :, :], scalar1=1.0 / 256, scalar2=None, op0=ALU.mult),
            [("ksum",)], [("kmean",)])
        proj_heads(False, rslot(1), cp)
        ring_load(gp0 + 1 + RING)
        if ci + 1 < total_chunks:
            dma(ropeb[0:64, :, :], rope_d[:, (ci + 1) % NCH, :, :], [], [("rope",)], "rope")
        for t in range(4):
            proj_v(t, rslot(2), cp)
        ring_load(gp0 + 2 + RING)
        gating_part1(cp)
        for g in range(4):
            proj_u(g, rslot(3), cp)
        ring_load(gp0 + 3 + RING)
        dump("Kc", Kc[:], [("K", h) for h in range(8)])
        dump("Vc", Vc[:], [("V", kt) for kt in range(16)])
        if stop <= 3:
            raise _Stop()
        for t in range(4):
            gating_part2(t, cp)
        for g in range(4):
            pool_chain(g, cp)
        dump("Q", Q, [("Q", h) for h in range(8)] + QB_ALL)
        dump("dT", dT, [("dT", g) for g in range(4)])
        for bk in range(4, 8):
            hold(bk)
        fin = None
        for i in range(4):
            fin = attn_pair(i, cp, fin, last=(i == 3))
        fin()
        for bk in range(4, 8):
            release(bk)
        if stop <= 4:
            raise _Stop()
        for g in range(4):
            pool_mm(g)
        dump("cat", cat, CAT_ALL)
        for t in range(4):
            wout_tile(ci, t, [rslot(4), rslot(5)])
        ring_load(gp0 + 4 + RING)
        ring_load(gp0 + 5 + RING)
        for t in range(4):
            dump(f"x1_{t}", xs[(ci * 4 + t) % XSLOTS][:], [("x", (ci * 4 + t) % XSLOTS)])
        if stop <= 5:
            raise _Stop()
        norm_prep(ci)
        norm_tp(b, A2, 24)
        for m in range(32):
            wup_chunk(m, rslot(6 + m // 4))
            if m % 4 == 3:
                ring_load(gp0 + 6 + m // 4 + RING)
        if ci + 1 < total_chunks:
            norm_prep(ci + 1)
        for hf in range(2):
            for kg in range(4):
                for t in range(4):
                    wdown_group(hf, kg, t, rslot(14 + hf * 4 + kg))
                ring_load(gp0 + 14 + hf * 4 + kg + RING)
        s0 = epiA(ci, 0, [0, 4], 1)
        s1 = epiA(ci, 1, [1, 5], 1)
        if ci + 1 < total_chunks:
            norm_tp((ci + 1) // NCH, A1, 0, tpbanks=[0, 4, 1, 5])
        epiB(s0, True)
        epiB(s1, True)
        s2 = epiA(ci, 2, [2, 6], 1)
        s3 = epiA(ci, 3, [3, 7], 1)
        epiB(s2, True)
        epiB(s3, True)
        bank_state["pref"] = [0, 4, 1, 5]
        bank_state["next"] = 2

    try:
        if stop <= 0:
            raise _Stop()
        for ci in range(total_chunks):
            do_chunk(ci)
    except _Stop:
        pass

    sch.finalize()
    eng_sems = {e: es.enter_context(nc.semaphore("sem_" + e)) for e in ("pe", "act", "dve", "pool")}
    dma_sems = {k: es.enter_context(nc.semaphore("dsem_" + k)) for k in dma_keys}
    final_waits = [(dma_sems[k], 16 * sch.dma_cnt[k]) for k in dma_keys]
    block = es.enter_context(nc.Block())

    @block.sync
    def _(eng):
        sch.emit("sp", eng, eng_sems, dma_sems, final_waits)

    @block.tensor
    def _(eng):
        sch.emit("pe", eng, eng_sems, dma_sems)

    @block.scalar
    def _(eng):
        sch.emit("act", eng, eng_sems, dma_sems)

    @block.vector
    def _(eng):
        sch.emit("dve", eng, eng_sems, dma_sems)

    @block.gpsimd
    def _(eng):
        sch.emit("pool", eng, eng_sems, dma_sems)

    es.close()
    return nc


def _consts():
    cst = np.zeros((128, 6, 128), np.float32)
    cst[:, 0, :] = np.eye(128, dtype=np.float32)
    for m in range(64):
        cst[(m + 32) % 64, 1, m] = 1.0
        cst[64 + m, 1, 64 + m] = 1.0
        cst[64 + (m + 32) % 64, 5, m] = 1.0
    k = np.arange(128)[:, None]
    q = np.arange(128)[None, :]
    cst[:, 2, :] = (k <= q).astype(np.float32)
    cst[:, 3, :] = 1.0
    cst[:, 4, :] = np.where(k <= q, 0.0, NEG).astype(np.float32)
    half = 32
    inv_freq = (1.0 / (10000.0 ** (np.arange(half, dtype=np.float32) * np.float32(2.0 / 64)))).astype(np.float32)
    ang = np.arange(S, dtype=np.float32)[None, :] * inv_freq[:, None]
    cos = np.cos(ang).astype(np.float32)
    sin = np.sin(ang).astype(np.float32)
    cos64 = np.concatenate([cos, cos], 0)
    sin64 = np.concatenate([-sin, sin], 0)
    rope = np.stack([cos64.reshape(64, NCH, T), sin64.reshape(64, NCH, T)], axis=2)
    kinit = np.zeros((128, S), np.float32)
    blk = np.arange(S) // 256
    for j in range(8):
        kinit[64 + j, :] = (blk == j).astype(np.float32)
    invc = np.zeros((128, 4, 16), np.float32)
    for g, w in enumerate((2, 4, 8, 16)):
        invc[:, g, :] = 1.0 / np.minimum(np.arange(16) + 1, w)
    return cst, np.ascontiguousarray(rope), kinit, invc


def _piece(w, r0, c0):
    blk = w[r0:r0 + 1024, c0:c0 + 512].reshape(8, 128, 512).transpose(1, 0, 2)
    return np.ascontiguousarray(blk).reshape(128, 4096)


def _prep_shared(inp):
    f = lambda a: np.asarray(a, dtype=np.float32)
    w_in, w_out, w_up, w_down = f(inp["w_in"])[0], f(inp["w_out"])[0], f(inp["w_up"])[0], f(inp["w_down"])[0]
    pieces = []
    pieces.append(_piece(w_in, 0, 512))
    pieces.append(_piece(w_in, 0, 0))
    pieces.append(_piece(w_in, 0, 1024))
    pieces.append(_piece(w_in, 0, 1536))
    for hf in range(2):
        pieces.append(_piece(w_out, 0, hf * 512))
    for j in range(8):
        pieces.append(_piece(w_up, 0, j * 512))
    for hf in range(2):
        for kg in range(4):
            pieces.append(_piece(w_down, kg * 1024, hf * 512))
    W = np.stack(pieces, 0)
    w_ada = f(inp["w_ada"])[0]
    wada = np.stack([_piece(w_ada, 0, pi * 512) for pi in range(12)], 0)
    bada = np.ascontiguousarray(f(inp["b_ada"])[0].reshape(48, 128).T)
    gvec = np.stack([f(inp[k])[0].reshape(8, 128).T for k in ("g_mix_pre", "g_mix_post", "g_mlp_pre", "g_mlp_post")], axis=1)
    pscale = np.ascontiguousarray(f(inp["pool_scale"])[0].reshape(4, 128).T)
    wpool = np.ascontiguousarray(f(inp["w_pool"])[0].transpose(1, 0, 2))
    cst, rope, kinit, invc = _consts()
    return dict(W=W, wada=wada, bada=bada, gvec=np.ascontiguousarray(gvec), pscale=pscale, wpool=wpool,
                cst=cst, rope=rope, kinit=kinit, invc=invc)


def _in_maps(inp, ncores=NCORES, nseq=NSEQ):
    shared = _prep_shared(inp)
    x = np.asarray(inp["x"], dtype=np.float32)
    c = np.asarray(inp["c"], dtype=np.float32)
    maps = []
    for i in range(ncores):
        xb = np.ascontiguousarray(x[i * NSEQ:i * NSEQ + nseq].reshape(nseq * S, D))
        cb = c[i * NSEQ:(i + 1) * NSEQ]
        cT = np.ascontiguousarray(cb.reshape(NSEQ, 8, 128).transpose(2, 1, 0))
        m = dict(shared)
        m["x"] = xb
        m["cT"] = cT
        maps.append(m)
    return maps


def kernel(**inputs):
    nc = bass.Bass("TRN2", target_bir_lowering=False)
    build(nc)
    maps = _in_maps(inputs)
    res = run_bass_kernel_spmd(nc, maps, core_ids=list(range(NCORES)))
    outs = [np.asarray(r["out"]).reshape(NSEQ, S, D) for r in res.results]
    return np.concatenate(outs, axis=0).astype(np.float32)
```

```python
import numpy as np
from contextlib import ExitStack
import concourse.bass as bass
import concourse.mybir as mybir
from concourse.bass_utils import run_bass_kernel_spmd

F32 = mybir.dt.float32
BF16 = mybir.dt.bfloat16
ALU = mybir.AluOpType
AF = mybir.ActivationFunctionType
AX = mybir.AxisListType

NCORES = 8
NSEQ = 4
S = 2048
D = 1024
T = 512
NCH = S // T
NPIECE = 22
RING = 4
XSLOTS = 8
EPS = 1e-6
NEG = -30000.0
PAIR_EXP = False


class Op:
    __slots__ = ("eng", "fn", "deps", "inc", "count", "dma", "dma_val", "name")


class Sched:
    ENGS = ("pe", "act", "dve", "pool", "sp")

    def __init__(self):
        self.ops = {e: [] for e in self.ENGS}
        self.lastw = {}
        self.readers = {}
        self.dma_cnt = {}

    def add(self, eng, fn, reads=(), writes=(), dma=None, name=""):
        op = Op()
        op.eng, op.fn, op.inc, op.count, op.dma, op.dma_val, op.name = eng, fn, False, 0, dma, 0, name
        deps = []
        seen = set()

        def push(o):
            if o is not None and id(o) not in seen:
                seen.add(id(o))
                deps.append(o)

        for r in reads:
            push(self.lastw.get(r))
        for w in writes:
            push(self.lastw.get(w))
            for o in self.readers.get(w, {}).values():
                push(o)
        op.deps = [d for d in deps if not (d.eng == "pe" and eng == "pe")]
        for d in op.deps:
            if d.dma is None:
                d.inc = True
        for r in reads:
            self.readers.setdefault(r, {})[eng] = op
        for w in writes:
            self.lastw[w] = op
            self.readers[w] = {}
        if dma is not None:
            self.dma_cnt[dma] = self.dma_cnt.get(dma, 0) + 1
            op.dma_val = 16 * self.dma_cnt[dma]
        self.ops[eng].append(op)
        return op

    def finalize(self):
        for e in self.ENGS:
            c = 0
            for op in self.ops[e]:
                if op.inc:
                    c += 1
                    op.count = c

    def emit(self, e, eng, eng_sems, dma_sems, final_waits=()):
        waited = {}
        for op in self.ops[e]:
            for d in op.deps:
                if d.dma is not None:
                    key, val, sem = ("d", d.dma), d.dma_val, dma_sems[d.dma]
                else:
                    key, val, sem = ("e", d.eng), d.count, eng_sems[d.eng]
                if waited.get(key, 0) < val:
                    eng.wait_ge(sem, val)
                    waited[key] = val
            ins = op.fn(eng)
            if op.dma is not None:
                ins.then_inc(dma_sems[op.dma], 16)
            elif op.inc:
                ins.then_inc(eng_sems[e], 1)
        for (sem, val) in final_waits:
            eng.wait_ge(sem, val)


def _alias(key):
    k0 = key[0]
    if k0 == "hid":
        m = key[1]
        if m < 8:
            return [("Q", m)] + [("Qb", t) for t in range(4)] + [("Qb", t, x) for t in range(4) for x in range(2)]
        if m < 12:
            return [("cat", m - 8, 0), ("cat", m - 8, 1)]
        if m < 16:
            return [("cat", m - 8)]
        if m < 20:
            return [("dT", m - 16)]
        out = []
        if m <= 28:
            out += [("ue", g) for g in range(4)]
        if m >= 28:
            out += [("pbuf", 0), ("pbuf", 1)]
        return out
    if k0 == "Q":
        return [("hid", key[1])]
    if k0 == "Qb":
        if len(key) == 3:
            return [("hid", h) for h in range(4 * key[2], 4 * key[2] + 4)]
        return [("hid", h) for h in range(8)]
    if k0 == "cat":
        return [("hid", 8 + key[1])]
    if k0 == "dT":
        return [("hid", 16 + key[1])]
    if k0 == "ue":
        return [("hid", m) for m in range(20, 29)]
    if k0 == "pbuf":
        return [("hid", m) for m in range(28, 32)]
    if k0 == "stg":
        return [("hid", m) for m in range(16 * key[1], 16 * key[1] + 16)]
    return []


def _expand(keys):
    out = []
    seen = set()
    for k in keys:
        for kk in [k] + _alias(k):
            if kk not in seen:
                seen.add(kk)
                out.append(kk)
    return out


class _Stop(Exception):
    pass


def build(nc, nseq=NSEQ, nchunks=None, dumps=(), stop=99):
    total_chunks = nseq * NCH if nchunks is None else nchunks
    ntok = nseq * S
    es = ExitStack()
    sch = Sched()

    def add(eng, fn, reads=(), writes=(), dma=None, name=""):
        return sch.add(eng, fn, _expand(list(reads)), _expand(list(writes)), dma=dma, name=name)

    def dram(name, shape, dt, kind):
        return nc.dram_tensor(name, list(shape), dt, kind=kind).ap()

    x_d = dram("x", [ntok, D], F32, "ExternalInput")
    out_d = dram("out", [ntok, D], F32, "ExternalOutput")
    cT_d = dram("cT", [128, 8, NSEQ], F32, "ExternalInput")
    wada_d = dram("wada", [12, 128, 4096], F32, "ExternalInput")
    bada_d = dram("bada", [128, 48], F32, "ExternalInput")
    gvec_d = dram("gvec", [128, 4, 8], F32, "ExternalInput")
    pscale_d = dram("pscale", [128, 4], F32, "ExternalInput")
    wpool_d = dram("wpool", [128, 4, 128], F32, "ExternalInput")
    W_d = dram("W", [NPIECE, 128, 4096], F32, "ExternalInput")
    cst_d = dram("cst", [128, 6, 128], F32, "ExternalInput")
    rope_d = dram("rope", [64, NCH, 2, T], F32, "ExternalInput")
    kinit_d = dram("kinit", [128, S], F32, "ExternalInput")
    invc_d = dram("invc", [128, 4, 16], F32, "ExternalInput")
    wbf_d = dram("wbf", [NPIECE, 128, 4096], BF16, "Internal")
    dump_d = {}
    for (nm, shape, dt) in dumps:
        dump_d[nm] = dram("dbg_" + nm, shape, dt, "ExternalOutput")

    def sb(name, shape, dt):
        return es.enter_context(nc.sbuf_tensor(name, list(shape), dt))

    xs = [sb(f"xs{i}", [128, D], F32) for i in range(XSLOTS)]
    ring = [sb(f"ring{i}", [128, 8, 512], BF16) for i in range(RING)]
    Kc = sb("Kc", [128, 8, S], BF16)
    Vc = sb("Vc", [128, 16, 4, 160], BF16)
    xn = [sb(f"xn{i}", [128, D], BF16) for i in range(4)]
    hT = sb("hT", [128, 8, T], BF16)
    big = sb("big", [128, 33 * 512], BF16)
    bigv = big[:]
    hid = bigv[:, 0:32 * 512].rearrange("p (m t) -> p m t", m=32)
    Q = bigv[:, 0:4096].rearrange("p (h t) -> p h t", h=8)
    cat = bigv[:, 4096:8192].rearrange("p (h t) -> p h t", h=8)
    dT = bigv[:, 8192:10240].rearrange("p (g t) -> p g t", g=4)
    ue = bigv[:, 10240:14464].bitcast(F32).rearrange("p (g t) -> p g t", g=4)
    pbuf = [bigv[:, 14464:15520].bitcast(F32), bigv[:, 15520:16576].bitcast(F32)]
    stgf = [bigv[:, 0:8192].bitcast(F32), bigv[:, 8192:16384].bitcast(F32)]
    PT = [sb(f"PT{i}", [128, T], BF16) for i in range(4)]
    G = [sb(f"G{i}", [128, D], F32) for i in range(2)]
    ropeb = sb("ropeb", [128, 2, T], F32)
    biasT = [sb(f"biasT{i}", [128, 8, 72], BF16) for i in range(4)]
    junk = sb("junk", [128, D], BF16)
    qraw = [sb(f"qraw{i}", [128, T], BF16) for i in range(2)]
    tf = [sb(f"tf{i}", [128, T], F32) for i in range(4)]
    bcs = sb("bcs", [128, T], F32)
    rr = sb("rr", [128, T], BF16)
    gm = sb("gm", [128, 8, 8], F32)
    cmpb = bcs[:].rearrange("p (a b) -> p a b", b=8)
    rank = sb("rank", [128, 8, 8], F32)
    kmean = sb("kmean", [128, 8, 8], BF16)
    ksum = sb("ksum", [128, 8, 2], F32)
    identf_t = sb("identf", [128, 128], F32)
    cstb = sb("cstb", [128, 6, 128], BF16)
    wpb = sb("wpb", [128, 4, 128], BF16)
    cTs = sb("cTs", [128, 8, NSEQ], F32)
    cact = sb("cact", [128, 8, NSEQ], F32)
    bada = sb("badas", [128, 48], F32)
    gvec = sb("gvecs", [128, 4, 8], F32)
    pscale = sb("pscales", [128, 4], F32)
    invc = sb("invcs", [128, 4, 16], F32)
    modT = sb("modT", [128, 48, NSEQ], F32)
    A1 = sb("A1", [128, 8, NSEQ], F32)
    A2 = sb("A2", [128, 8, NSEQ], F32)
    GA = [sb(f"GA{i}", [128, 8, NSEQ], F32) for i in range(2)]
    gcol = [sb("gcol0", [128, 128], F32)] * 2
    stat = sb("stat", [128, 64], F32)
    tmpf = sb("tmpf", [128, 16], F32)
    epsb = sb("epsb", [128, 1], F32)
    halo = sb("halo", [128, 4, 16], F32)

    psall_t = es.enter_context(nc.psum_tensor("psall", [128, 8 * 512], F32))
    psall = psall_t[:]
    ps = [psall[:, i * 512:(i + 1) * 512] for i in range(8)]

    identb = cstb[:, 0, :]
    rsw = cstb[0:64, 1, 0:64]
    tri = cstb[:, 2, :]
    trineg = cstb[:, 4, :]
    identf = identf_t[:]
    cstf = stgf[0][:, 0:768].rearrange("p (a b) -> p a b", a=6)
    wpf = stgf[0][:, 1024:1536].rearrange("p (a b) -> p a b", a=4)

    bank_state = {"next": 0, "held": set(), "pref": []}

    def alloc_bank():
        while bank_state["pref"]:
            b = bank_state["pref"].pop(0)
            if b not in bank_state["held"]:
                return b
        for _ in range(16):
            b = bank_state["next"]
            bank_state["next"] = (b + 1) % 8
            if b not in bank_state["held"]:
                return b
        raise RuntimeError("no free psum bank")

    def hold(b):
        bank_state["held"].add(b)

    def release(b):
        bank_state["held"].discard(b)

    stat_i = [0]

    def new_stat(n=1):
        i = stat_i[0]
        if i + n > 64:
            i = 0
        stat_i[0] = i + n
        return i

    dma_keys = []

    def dma(out, in_, reads, writes, key):
        if key not in dma_keys:
            dma_keys.append(key)
        return add("sp", lambda e: e.dma_start(out=out, in_=in_), reads=reads, writes=writes, dma=key)

    def dump(nm, src_ap, reads):
        if nm in dump_d:
            dma(dump_d[nm], src_ap, reads, [("dump", nm)], "dump_" + nm)

    def mm_group(out, pairs):
        def f(e):
            ins = None
            n = len(pairs)
            for i, (l, r) in enumerate(pairs):
                ins = e.matmul(out=out, lhsT=l, rhs=r, start=(i == 0), stop=(i == n - 1))
            return ins
        return f

    HT_ALL = [("hT", j) for j in range(8)]
    QB_ALL = [("Qb", t) for t in range(4)] + [("Qb", t, x) for t in range(4) for x in range(2)]
    CAT_ALL = [("cat", i, x) for i in range(4) for x in range(2)] + [("cat", 4 + g) for g in range(4)]

    dma(cstf, cst_d, [], [("stg", 0)], "stg0")
    dma(cTs[:], cT_d, [], [("cT",)], "c1")
    dma(bada[:], bada_d, [], [("bada",)], "c2")
    dma(gvec[:], gvec_d, [], [("gvec",)], "c3")
    dma(pscale[:], pscale_d, [], [("pscale",)], "c4")
    dma(invc[:], invc_d, [], [("invc",)], "c5")
    dma(wpf, wpool_d, [("stg", 0)], [("stg", 0)], "stg0")
    add("dve", lambda e: e.tensor_copy(out=cstb[:], in_=cstf), [("stg", 0)], [("cstb",)])
    add("dve", lambda e: e.tensor_copy(out=wpb[:], in_=wpf), [("stg", 0)], [("wpb",)])
    add("dve", lambda e: e.tensor_copy(out=identf, in_=cstf[:, 0, :]), [("stg", 0)], [("cstf",)])
    add("act", lambda e: e.activation(out=cact[:], in_=cTs[:], func=AF.Silu), [("cT",)], [("cact",)])
    add("pool", lambda e: e.memset(epsb[:], EPS), [], [("epsb",)])

    def conv_piece(pi):
        sv = stgf[1]
        rs = pi % RING
        dma(sv, W_d[pi], [], [("stg", 1)], "stg1")
        rv = ring[rs][:].rearrange("p k c -> p (k c)")
        add("dve", lambda e: e.tensor_copy(out=rv[:, 0:2048], in_=sv[:, 0:2048]), [("stg", 1)], [("ringh", rs, 0)])
        add("act", lambda e: e.copy(out=rv[:, 2048:4096], in_=sv[:, 2048:4096]), [("stg", 1)], [("ringh", rs, 1)])
        dma(wbf_d[pi], rv, [("ringh", rs, 0), ("ringh", rs, 1)], [("wbf", pi), ("ring", rs)], f"ring{rs}")

    modbank = alloc_bank()
    hold(modbank)

    def mod_piece(pi):
        sv = stgf[0]
        dma(sv, wada_d[pi], [], [("stg", 0)], "stg0")
        svv = sv.rearrange("p (k c) -> p k c", k=8)

        def f(e):
            ins = None
            for fc in range(4):
                col = (pi * 4 + fc) * NSEQ
                for kk in range(8):
                    ins = e.matmul(out=ps[modbank][:, col:col + NSEQ], lhsT=svv[:, kk, fc * 128:(fc + 1) * 128],
                                   rhs=cact[:, kk, :], start=(kk == 0), stop=(kk == 7))
            return ins
        add("pe", f, [("stg", 0), ("cact",)], [("ps", modbank)])
    ci_ = 0
    for pi in range(12):
        mod_piece(pi)
        for _ in range(2):
            if ci_ < NPIECE:
                conv_piece(ci_)
                ci_ += 1
    add("dve", lambda e: e.tensor_tensor(out=modT[:], in0=ps[modbank][:, 0:48 * NSEQ].rearrange("p (f b) -> p f b", b=NSEQ),
                                        in1=bada[:].unsqueeze(2).broadcast_to([128, 48, NSEQ]), op=ALU.add),
        [("ps", modbank), ("bada",)], [("modT",)])
    release(modbank)

    def gb(w):
        return gvec[:, w, :].unsqueeze(2).broadcast_to([128, 8, NSEQ])
    add("dve", lambda e: e.scalar_tensor_tensor(out=A1[:], in0=modT[:, 8:16, :], scalar=1.0, in1=gb(0), op0=ALU.add, op1=ALU.mult),
        [("modT",), ("gvec",)], [("A1",)])
    add("dve", lambda e: e.scalar_tensor_tensor(out=A2[:], in0=modT[:, 32:40, :], scalar=1.0, in1=gb(2), op0=ALU.add, op1=ALU.mult),
        [("modT",), ("gvec",)], [("A2",)])
    add("dve", lambda e: e.tensor_tensor(out=GA[0][:], in0=modT[:, 16:24, :], in1=gb(1), op=ALU.mult), [("modT",), ("gvec",)], [("GA", 0)])
    add("dve", lambda e: e.tensor_tensor(out=GA[1][:], in0=modT[:, 40:48, :], in1=gb(3), op=ALU.mult), [("modT",), ("gvec",)], [("GA", 1)])

    kin = stgf[0][:, 0:S]
    dma(kin, kinit_d, [], [("stg", 0)], "stg0")

    def kinit_head(h):
        if h % 2 == 0:
            add("dve", lambda e: e.tensor_copy(out=Kc[:, h, :], in_=kin), [("stg", 0)], [("K", h)])
        else:
            add("act", lambda e: e.copy(out=Kc[:, h, :], in_=kin), [("stg", 0)], [("K", h)])
    for h in range(8):
        kinit_head(h)
    add("pool", lambda e: e.memset(Vc[:], 0.0), [], [("Vinit",)])
    add("pool", lambda e: e.memset(Vc[:, :, :, 64:65], 1.0), [("Vinit",)], [("V", kt) for kt in range(16)])
    add("pool", lambda e: e.memset(kmean[:], 0.0), [], [("kmean",)])

    def ring_load(gp):
        if gp >= total_chunks * NPIECE:
            return
        pi = gp % NPIECE
        s_ = gp % RING
        dma(ring[s_][:].rearrange("p k c -> p (k c)"), wbf_d[pi], [("wbf", pi)], [("ring", s_)], f"ring{s_}")

    def xload(tg):
        if tg >= total_chunks * 4:
            return
        sl = tg % XSLOTS
        dma(xs[sl][:], x_d[tg * 128:(tg + 1) * 128, :], [], [("x", sl)], f"x{sl}")

    for gp in range(RING):
        ring_load(gp)
    for tg in range(XSLOTS):
        xload(tg)

    def rstd_from(ss_ap, reads):
        i1 = new_stat()
        i2 = new_stat()
        add("act", lambda e: e.activation(out=stat[:, i1:i1 + 1], in_=ss_ap, func=AF.Ln, scale=1.0 / D, bias=epsb[:, 0:1]),
            list(reads) + [("epsb",)], [("st", i1)])
        add("act", lambda e: e.activation(out=stat[:, i2:i2 + 1], in_=stat[:, i1:i1 + 1], func=AF.Exp, scale=-0.5),
            [("st", i1)], [("st", i2)])
        return i2

    def norm_prep_tile(ci, t):
        sl = (ci * 4 + t) % XSLOTS
        iss = new_stat()
        add("act", lambda e: e.activation(out=junk[:], in_=xs[sl][:], func=AF.Square, accum_out=stat[:, iss:iss + 1]),
            [("x", sl)], [("st", iss), ("junk", 0), ("junk", 1)])
        ir = rstd_from(stat[:, iss:iss + 1], [("st", iss)])
        add("dve", lambda e: e.tensor_scalar(out=xn[t][:], in0=xs[sl][:], scalar1=stat[:, ir:ir + 1], scalar2=None, op0=ALU.mult),
            [("x", sl), ("st", ir)], [("xn", t)])

    def norm_prep(ci):
        for t in range(4):
            norm_prep_tile(ci, t)

    def tp_tile(t, tpbanks):
        def f(e):
            ins = None
            for j in range(8):
                bk = ps[tpbanks[j // 2]][:].bitcast(BF16)
                pos = (j % 2) * 4 + t
                ins = e.transpose(out=bk[:, pos * 128:(pos + 1) * 128], in_=xn[t][:, j * 128:(j + 1) * 128], identity=identb)
            return ins
        add("pe", f, [("xn", t), ("cstb",)], [("ps", tpbanks[jj]) for jj in range(4)])

    def evac_h(j, b, Amat, shbase, tpbanks):
        bk = ps[tpbanks[j // 2]][:].bitcast(BF16)[:, (j % 2) * 512:(j % 2 + 1) * 512]
        sc = Amat[:, j, b:b + 1]
        bi = modT[:, shbase + j, b:b + 1]
        rd = [("ps", tpbanks[j // 2]), ("A1",), ("A2",), ("modT",)]
        if (j // 2) % 2 == 0:
            add("act", lambda e: e.activation(out=hT[:, j, :], in_=bk, func=AF.Identity, scale=sc, bias=bi), rd, [("hT", j)])
        else:
            add("dve", lambda e: e.tensor_scalar(out=hT[:, j, :], in0=bk, scalar1=sc, scalar2=bi, op0=ALU.mult, op1=ALU.add), rd, [("hT", j)])

    def norm_tp(b, Amat, shbase, tpbanks=None):
        if tpbanks is None:
            tpbanks = [alloc_bank() for _ in range(4)]
        for t in range(4):
            tp_tile(t, tpbanks)
        for j in range(8):
            evac_h(j, b, Amat, shbase, tpbanks)

    def epiA(ci, t, banks, Gi):
        i0 = new_stat(2)
        tbs = [(2 * t) % 4, (2 * t + 1) % 4]

        def sq(hf):
            add("act", lambda e: e.activation(out=junk[:, hf * 512:(hf + 1) * 512], in_=ps[banks[hf]][:], func=AF.Square, accum_out=stat[:, i0 + hf:i0 + hf + 1]),
                [("ps", banks[hf])], [("st", i0 + hf), ("junk", hf)])

        def mul(hf):
            tb = tbs[hf]
            add("dve", lambda e: e.tensor_tensor(out=tf[tb][:], in0=ps[banks[hf]][:], in1=G[Gi][:, hf * 512:(hf + 1) * 512], op=ALU.mult),
                [("ps", banks[hf]), ("G", Gi), ("st", i0 + hf)], [("tf", tb)])
        sq(0)
        sq(1)
        mul(0)
        mul(1)
        return (ci, t, i0, tbs)

    def epiB(st, store):
        ci, t, i0, tbs = st
        sl = (ci * 4 + t) % XSLOTS
        isum = new_stat()
        add("dve", lambda e: e.tensor_tensor(out=stat[:, isum:isum + 1], in0=stat[:, i0:i0 + 1], in1=stat[:, i0 + 1:i0 + 2], op=ALU.add),
            [("st", i0), ("st", i0 + 1)], [("st", isum)])
        ir = rstd_from(stat[:, isum:isum + 1], [("st", isum)])

        def stt(hf):
            tb = tbs[hf]
            add("dve", lambda e: e.scalar_tensor_tensor(out=xs[sl][:, hf * 512:(hf + 1) * 512], in0=tf[tb][:], scalar=stat[:, ir:ir + 1],
                                                       in1=xs[sl][:, hf * 512:(hf + 1) * 512], op0=ALU.mult, op1=ALU.add),
                [("tf", tb), ("st", ir), ("x", sl)], [("x", sl)])
        stt(0)
        stt(1)
        if store:
            tg = ci * 4 + t
            dma(out_d[tg * 128:(tg + 1) * 128, :], xs[sl][:], [("x", sl)], [("out", tg)], f"x{sl}")
            xload(tg + XSLOTS)

    def epilogue(ci, t, banks, Gi, store):
        epiB(epiA(ci, t, banks, Gi), store)

    def seq_setup(b):
        def one(w, j, gbanks):
            gi = 0
            add("dve", lambda e: e.tensor_copy(out=gcol[gi][:], in_=GA[w][:, j, b:b + 1].broadcast_to([128, 128])),
                [("GA", w)], [("gcol", gi)])
            add("pe", lambda e: e.matmul(out=ps[gbanks[j // 4]][:, (j % 4) * 128:(j % 4 + 1) * 128], lhsT=gcol[gi][:], rhs=identf, start=True, stop=True),
                [("gcol", gi), ("cstf",)], [("ps", gbanks[j // 4])])

        def ev(w, hf, gbanks):
            add("act", lambda e: e.copy(out=G[w][:, hf * 512:(hf + 1) * 512], in_=ps[gbanks[hf]][:]), [("ps", gbanks[hf])], [("G", w)])
        for w in range(2):
            gbanks = [alloc_bank(), alloc_bank()]
            for j in range(8):
                one(w, j, gbanks)
            for hf in range(2):
                ev(w, hf, gbanks)
        add("pool", lambda e: e.memset(halo[:], 0.0), [], [("halo", g) for g in range(4)])
        add("pool", lambda e: e.memset(gm[:], -1e30), [], [("gm",)])

        def zb(t):
            add("pool", lambda e: e.memset(biasT[t][:], 0.0), [], [("biasT", t)])
        for t in range(4):
            zb(t)

    def proj_step1(i, is_k, rs, cp):
        pbank = alloc_bank()
        add("pe", mm_group(ps[pbank][:], [(ring[rs][:, kk, i * 128:(i + 1) * 128], hT[:, kk, :]) for kk in range(8)]),
            [("ring", rs)] + HT_ALL, [("ps", pbank)])
        qb = i % 2
        add("act", lambda e: e.copy(out=qraw[qb][:], in_=ps[pbank][:]), [("ps", pbank)], [("qraw", qb)])
        return (i, is_k, cp, qb, pbank)

    def proj_step2(st):
        i, is_k, cp, qb, pbank = st
        bR, bS, bT = alloc_bank(), alloc_bank(), alloc_bank()
        add("dve", lambda e: e.tensor_tensor(out=tf[0][0:64, :], in0=ps[pbank][0:64, :], in1=ropeb[0:64, 0, :], op=ALU.mult),
            [("ps", pbank), ("rope",), ("qraw", qb)], [("tf", 0)])

        def f(e):
            e.matmul(out=ps[bR][0:64, :], lhsT=cstb[:, 1, 0:64], rhs=qraw[qb][:], start=True, stop=True)
            e.matmul(out=ps[bS][0:64, :], lhsT=cstb[:, 1, 64:128], rhs=qraw[qb][:], start=True, stop=True)
            return e.matmul(out=ps[bT][0:64, :], lhsT=cstb[:, 5, 0:64], rhs=qraw[qb][:], start=True, stop=True)
        add("pe", f, [("qraw", qb), ("cstb",)], [("ps", bR), ("ps", bS), ("ps", bT)])
        add("dve", lambda e: e.tensor_tensor(out=tf[1][0:64, :], in0=ps[bR][0:64, :], in1=ropeb[0:64, 1, :], op=ALU.mult),
            [("ps", bR), ("rope",)], [("tf", 1)])
        add("dve", lambda e: e.tensor_tensor(out=tf[2][0:64, :], in0=ps[bS][0:64, :], in1=ropeb[0:64, 0, :], op=ALU.mult),
            [("ps", bS), ("rope",)], [("tf", 2)])
        add("dve", lambda e: e.tensor_tensor(out=tf[3][0:64, :], in0=ps[bT][0:64, :], in1=ropeb[0:64, 1, :], op=ALU.mult),
            [("ps", bT), ("rope",)], [("tf", 3)])
        for e_, (ta, tb_) in enumerate(((0, 1), (2, 3))):
            h = 2 * i + e_
            if is_k:
                dst = Kc[0:64, h, cp * T:(cp + 1) * T]
                wr = [("K", h)]
            else:
                dst = Q[0:64, h, :]
                wr = [("Q", h)]
            rope_add(dst, ta, tb_, wr)

    def rope_add(dst, ta, tb_, wr):
        add("pool", lambda e: e.tensor_tensor(out=dst, in0=tf[ta][0:64, :], in1=tf[tb_][0:64, :], op=ALU.add),
            [("tf", ta), ("tf", tb_)], wr)

    def proj_heads(is_k, rs, cp):
        prev = None
        for i in range(4):
            st = proj_step1(i, is_k, rs, cp)
            if prev is not None:
                proj_step2(prev)
            prev = st
        proj_step2(prev)

    def proj_v(t, rs, cp):
        vbank = alloc_bank()
        kt = cp * 4 + t
        add("pe", mm_group(ps[vbank][:], [(hT[:, kk, t * 128:(t + 1) * 128], ring[rs][:, kk, :]) for kk in range(8)]),
            [("ring", rs)] + HT_ALL, [("ps", vbank)])
        vv = ps[vbank][:].rearrange("p (i e d) -> p i e d", i=4, e=2)
        add("act", lambda e: e.copy(out=Vc[:, kt, :, 0:64], in_=vv[:, :, 0, :]), [("ps", vbank)], [("V", kt)])
        add("dve", lambda e: e.tensor_copy(out=Vc[:, kt, :, 96:160], in_=vv[:, :, 1, :]), [("ps", vbank), ("V", kt)], [("V", kt)])

    def proj_u(g, rs, cp):
        ubank = alloc_bank()
        add("pe", mm_group(ps[ubank][:], [(ring[rs][:, kk, g * 128:(g + 1) * 128], hT[:, kk, :]) for kk in range(8)]),
            [("ring", rs)] + HT_ALL, [("ps", ubank)])
        add("act", lambda e: e.copy(out=ue[:, g, 16:528], in_=ps[ubank][:]), [("ps", ubank)], [("ue", g)])

    def pool_chain(g, cp):
        add("pool", lambda e: e.tensor_copy(out=ue[:, g, 0:16], in_=halo[:, g, :]), [("halo", g), ("ue", g)], [("ue", g)])

        def level(lv):
            sh = 1 << lv
            lo = (1 << (lv + 1)) - 1
            dstb = pbuf[lv % 2]
            if lv == 0:
                a0, a1 = ue[:, g, lo:528], ue[:, g, lo - sh:528 - sh]
                rd = [("ue", g)]
            else:
                sb_ = pbuf[(lv - 1) % 2]
                a0, a1 = sb_[:, lo:528], sb_[:, lo - sh:528 - sh]
                rd = [("pbuf", (lv - 1) % 2)]
            add("pool", lambda e: e.tensor_tensor(out=dstb[:, lo:528], in0=a0, in1=a1, op=ALU.add), rd, [("pbuf", lv % 2)])
        for lv in range(g + 1):
            level(lv)
        Lb = pbuf[g % 2]
        w = 1 << (g + 1)
        add("dve", lambda e: e.scalar_tensor_tensor(out=dT[:, g, :], in0=Lb[:, 16:528], scalar=1.0 / w, in1=ue[:, g, 16:528], op0=ALU.mult, op1=ALU.subtract),
            [("pbuf", g % 2), ("ue", g)], [("dT", g)])
        if cp == 0:
            add("dve", lambda e: e.tensor_tensor(out=tmpf[:, 0:15], in0=Lb[:, 16:31], in1=invc[:, g, 0:15], op=ALU.mult),
                [("pbuf", g % 2), ("invc",)], [("tmpf",)])
            add("dve", lambda e: e.tensor_tensor(out=dT[:, g, 0:15], in0=tmpf[:, 0:15], in1=ue[:, g, 16:31], op=ALU.subtract),
                [("tmpf",), ("ue", g), ("dT", g)], [("dT", g)])
        add("pool", lambda e: e.tensor_copy(out=halo[:, g, :], in_=ue[:, g, 512:528]), [("ue", g)], [("halo", g)])

    def gating_part1(cp):
        tiles = [t for t in range(4) if 2 * cp + t // 2 > 0]
        if not tiles:
            return
        gbanks = [alloc_bank(), alloc_bank()]
        for t in tiles:
            gating_scores(t, cp, gbanks[t % 2])

    def gating_scores(t, cp, gbank):
        blk = 2 * cp + t // 2
        c0, c1 = t * 128, (t + 1) * 128

        def f(e):
            ins = None
            for h in range(8):
                ins = e.matmul(out=ps[gbank][:, t * 64 + h * 8:t * 64 + (h + 1) * 8], lhsT=Q[0:64, h, c0:c1], rhs=kmean[0:64, h, :], start=True, stop=True)
            return ins
        add("pe", f, [("Q", h) for h in range(8)] + [("kmean",)], [("ps", gbank)])
        add("dve", lambda e: e.tensor_copy(out=gm[:, :, 0:blk], in_=ps[gbank][:, t * 64:(t + 1) * 64].rearrange("p (h j) -> p h j", h=8)[:, :, 0:blk]),
            [("ps", gbank)], [("gm",)])
        add("dve", lambda e: e.tensor_tensor(out=cmpb.rearrange("p (h j) i -> p h j i", h=8),
                                            in0=gm[:].unsqueeze(2).broadcast_to([128, 8, 8, 8]),
                                            in1=gm[:].unsqueeze(3).broadcast_to([128, 8, 8, 8]), op=ALU.is_gt),
            [("gm",)], [("bcs",)])
        add("dve", lambda e: e.tensor_reduce(out=rank[:].rearrange("p h j -> p (h j)"), in_=cmpb, axis=AX.X, op=ALU.add),
            [("bcs",)], [("rank",)])
        add("dve", lambda e: e.tensor_scalar(out=biasT[t][:, :, 64:64 + blk], in0=rank[:, :, 0:blk], scalar1=2.5, scalar2=NEG, op0=ALU.is_gt, op1=ALU.mult),
            [("rank",)], [("biasT", t)])

    def gating_part2(t, cp):
        blk = 2 * cp + t // 2
        c0, c1 = t * 128, (t + 1) * 128
        if blk == 0:
            add("pool", lambda e: e.memset(Q[64:72, :, c0:c1], 0.0), [], [("Qb", t)])
            return
        qbb = [alloc_bank(), alloc_bank()]

        def f2(e):
            ins = None
            for h in range(8):
                ins = e.matmul(out=ps[qbb[h // 4]][0:72, (h % 4) * 128:(h % 4 + 1) * 128], lhsT=biasT[t][:, h, :], rhs=identb, start=True, stop=True)
            return ins
        add("pe", f2, [("biasT", t), ("cstb",)], [("ps", qbb[0]), ("ps", qbb[1])])
        add("act", lambda e: e.copy(out=Q[64:72, 0:4, c0:c1], in_=ps[qbb[0]][64:72, :].rearrange("p (h c) -> p h c", h=4)),
            [("ps", qbb[0])], [("Qb", t, 0)])
        add("dve", lambda e: e.tensor_copy(out=Q[64:72, 4:8, c0:c1], in_=ps[qbb[1]][64:72, :].rearrange("p (h c) -> p h c", h=4)),
            [("ps", qbb[1])], [("Qb", t, 1)])

    rrf = tf[0]
    ptc = [0]
    sslot = [0]
    accrot = [0]

    def attn_head(i, e_, acc, cp, hook=None):
        h = 2 * i + e_
        units = []
        k0 = 4 * cp
        if PAIR_EXP:
            for u in range(0, 4 * cp, 2):
                units.append([(u, 0, 0, 0, 0), (u + 1, 0, 1, 0, 512)])
            units.append([(k0, 0, 0, 0, 0), (k0 + 1, 128, 1, 0, 512)])
            units.append([(k0 + 2, 256, 0, 0, 0), (k0 + 3, 384, 0, 256, 256)])
        else:
            for u in range(0, 4 * cp):
                units.append([(u, 0, 0, 0, 0)])
            for r in range(4):
                units.append([(k0 + r, 128 * r, 0, 0, 0)])
        nun = len(units)

        def emit_qk(u):
            if PAIR_EXP:
                slot = sslot[0] % 2
                b0 = 2 * slot
                raise RuntimeError("PAIR_EXP unsupported")
            else:
                b0 = sslot[0] % 4
                pb = ptc[0] % 4
                ptv = PT[pb]
            sslot[0] += 1
            ptc[0] += 1
            tl = units[u]
            wbanks = [("ps", b0), ("ps", b0 + 1)] if PAIR_EXP else [("ps", b0)]

            def f(e):
                ins = None
                for (kt, qlo, bi, c0, po) in tl:
                    N = T - qlo
                    diag = kt >= 4 * cp
                    ins = e.matmul(out=ps[b0 + bi][:, c0:c0 + N], lhsT=Kc[0:72, h, kt * 128:(kt + 1) * 128], rhs=Q[0:72, h, qlo:T], start=True, stop=(not diag))
                    if diag:
                        ins = e.matmul(out=ps[b0 + bi][:, c0:c0 + 128], lhsT=identb, rhs=trineg, start=False, stop=True)
                return ins
            add("pe", f, [("K", h), ("Q", h), ("cstb",)] + QB_ALL, wbanks)
            last = tl[-1]
            W = last[4] + (T - last[1])
            add("act", lambda e: e.activation(out=ptv[:, 0:W], in_=psall[:, b0 * 512:b0 * 512 + W], func=AF.Exp, scale=0.125),
                wbanks, [("PT", pb)])
            return (pb, ptv)

        def emit_pv(u, pbv):
            pb, ptv = pbv
            tl = units[u]

            def f(e):
                ins = None
                for ti, (kt, qlo, bi, c0, po) in enumerate(tl):
                    N = T - qlo
                    first = (u == 0 and ti == 0)
                    lastt = (u == nun - 1 and ti == len(tl) - 1)
                    if e_ == 0:
                        o = ps[acc][0:65, qlo:T]
                        l = Vc[:, kt, i, 0:65]
                    else:
                        o = ps[acc][:, qlo:T]
                        l = Vc[:, kt, i, 32:160]
                    ins = e.matmul(out=o, lhsT=l, rhs=ptv[:, po:po + N], start=first, stop=lastt)
                return ins
            add("pe", f, [("V", kt) for (kt, _, _, _, _) in tl] + [("PT", pb)], [("ps", acc)])

        pend = []
        for u in range(nun):
            pend.append((u, emit_qk(u)))
            if len(pend) > (1 if PAIR_EXP else 3):
                emit_pv(*pend.pop(0))
        while pend:
            emit_pv(*pend.pop(0))
        if hook is not None:
            hook()

    def attn_pair(i, cp, prev_finish, last=False):
        a0 = 4 + accrot[0] % 4
        a1 = 4 + (accrot[0] + 1) % 4
        accrot[0] += 2
        attn_head(i, 0, a0, cp, hook=prev_finish)
        attn_head(i, 1, a1, cp)

        def rcp(e, dst, src_):
            with nc.allow_low_precision("softmax normaliser broadcast through a bf16 K=1 matmul"):
                return e.reciprocal(out=dst, in_=src_)
        if last:
            def act_rcp(p0, a, k):
                add("act", lambda e: e.activation(out=rrf[p0:p0 + 1, :], in_=ps[a][p0:p0 + 1, :], func=AF.Ln), [("ps", a)], [("tf", 0)])
                add("act", lambda e: e.activation(out=rr[p0:p0 + 1, :], in_=rrf[p0:p0 + 1, :], func=AF.Exp, scale=-1.0), [("tf", 0)], [("rr", k)])
            act_rcp(64, a0, 0)
            act_rcp(32, a1, 1)
        else:
            add("dve", lambda e: rcp(e, rr[64:65, :], ps[a0][64:65, :]), [("ps", a0)], [("rr", 0)])
            add("dve", lambda e: rcp(e, rr[32:33, :], ps[a1][32:33, :]), [("ps", a1)], [("rr", 1)])

        def finish():
            bcb = 2 * (sslot[0] % 2) if PAIR_EXP else sslot[0] % 4
            sslot[0] += 1

            def fb(e):
                e.matmul(out=ps[bcb][0:64, :], lhsT=cstb[64:65, 3, 0:64], rhs=rr[64:65, :], start=True, stop=True)
                return e.matmul(out=ps[bcb][64:128, :], lhsT=cstb[32:33, 3, 0:64], rhs=rr[32:33, :], start=True, stop=True)
            add("pe", fb, [("rr", 0), ("rr", 1), ("cstb",)], [("ps", bcb)])
            add("dve", lambda e: e.tensor_copy(out=bcs[:], in_=ps[bcb][:]), [("ps", bcb)], [("bcs",)])
            add("dve", lambda e: e.tensor_tensor(out=cat[0:64, i, :], in0=ps[a0][0:64, :], in1=bcs[0:64, :], op=ALU.mult),
                [("ps", a0), ("bcs",)], [("cat", i, 0)])
            add("dve", lambda e: e.tensor_tensor(out=cat[64:128, i, :], in0=ps[a1][64:128, :], in1=bcs[64:128, :], op=ALU.mult),
                [("ps", a1), ("bcs",)], [("cat", i, 1)])
        return finish

    def pool_mm(g):
        pbk = alloc_bank()
        add("pe", lambda e: e.matmul(out=ps[pbk][:], lhsT=wpb[:, g, :], rhs=dT[:, g, :], start=True, stop=True),
            [("wpb",), ("dT", g)], [("ps", pbk)])
        add("dve", lambda e: e.tensor_scalar(out=cat[:, 4 + g, :], in0=ps[pbk][:], scalar1=pscale[:, g:g + 1], scalar2=None, op0=ALU.mult),
            [("ps", pbk), ("pscale",)], [("cat", 4 + g)])

    def wout_tile(ci, t, rs2):
        yb = [alloc_bank(), alloc_bank()]
        for hf in range(2):
            add("pe", mm_group(ps[yb[hf]][:], [(cat[:, kk, t * 128:(t + 1) * 128], ring[rs2[hf]][:, kk, :]) for kk in range(8)]),
                [("ring", rs2[hf])] + CAT_ALL, [("ps", yb[hf])])
        epilogue(ci, t, yb, 0, store=False)

    def wup_chunk(m, rs):
        hb = alloc_bank()
        add("pe", mm_group(ps[hb][:], [(ring[rs][:, kk, (m % 4) * 128:(m % 4 + 1) * 128], hT[:, kk, :]) for kk in range(8)]),
            [("ring", rs)] + HT_ALL, [("ps", hb)])
        rb_ = m % 2
        add("act", lambda e: e.activation(out=tf[rb_][:], in_=ps[hb][:], func=AF.Relu), [("ps", hb)], [("tf", rb_)])
        eng = "dve" if m % 2 == 0 else "pool"
        add(eng, lambda e: e.tensor_tensor(out=hid[:, m, :], in0=tf[rb_][:], in1=tf[rb_][:], op=ALU.mult), [("tf", rb_)], [("hid", m)])

    def wdown_group(hf, kg, t, rs):
        bk = hf * 4 + t

        def f(e):
            ins = None
            for kk in range(8):
                ins = e.matmul(out=ps[bk][:], lhsT=hid[:, kg * 8 + kk, t * 128:(t + 1) * 128], rhs=ring[rs][:, kk, :],
                               start=(kg == 0 and kk == 0), stop=(kg == 3 and kk == 7))
            return ins
        add("pe", f, [("ring", rs)] + [("hid", kg * 8 + kk) for kk in range(8)], [("ps", bk)])

    def do_chunk(ci):
        b = ci // NCH
        cp = ci % NCH
        gp0 = ci * NPIECE

        def rslot(pi):
            return (gp0 + pi) % RING

        if cp == 0:
            seq_setup(b)
        if ci == 0:
            dma(ropeb[0:64, :, :], rope_d[:, cp, :, :], [], [("rope",)], "rope")

        if stop <= 1:
            raise _Stop()
        if ci == 0:
            norm_prep(ci)
            norm_tp(b, A1, 0)
        dump("hT", hT[:], HT_ALL)
        if stop <= 2:
            raise _Stop()
        proj_heads(True, rslot(0), cp)
        ring_load(gp0 + 0 + RING)
        add("dve", lambda e: e.tensor_reduce(out=ksum[0:64, :, :], in_=Kc[0:64, :, cp * T:(cp + 1) * T].rearrange("p h (b t) -> p h b t", b=2), axis=AX.X, op=ALU.add),
            [("K", h) for h in range(8)], [("ksum",)])
        add("dve", lambda e: e.tensor_scalar(out=kmean[0:64, :, 2 * cp:2 * cp + 2], in0=ksum[0:64, :, :], scalar1=1.0 / 256, scalar2=None, op0=ALU.mult),
            [("ksum",)], [("kmean",)])
        proj_heads(False, rslot(1), cp)
        ring_load(gp0 + 1 + RING)
        if ci + 1 < total_chunks:
            dma(ropeb[0:64, :, :], rope_d[:, (ci + 1) % NCH, :, :], [], [("rope",)], "rope")
        for t in range(4):
            proj_v(t, rslot(2), cp)
        ring_load(gp0 + 2 + RING)
        gating_part1(cp)
        for g in range(4):
            proj_u(g, rslot(3), cp)
        ring_load(gp0 + 3 + RING)
        dump("Kc", Kc[:], [("K", h) for h in range(8)])
        dump("Vc", Vc[:], [("V", kt) for kt in range(16)])
        if stop <= 3:
            raise _Stop()
        for t in range(4):
            gating_part2(t, cp)
        for g in range(4):
            pool_chain(g, cp)
        dump("Q", Q, [("Q", h) for h in range(8)] + QB_ALL)
        dump("dT", dT, [("dT", g) for g in range(4)])
        for bk in range(4, 8):
            hold(bk)
        fin = None
        for i in range(4):
            fin = attn_pair(i, cp, fin, last=(i == 3))
        fin()
        for bk in range(4, 8):
            release(bk)
        if stop <= 4:
            raise _Stop()
        for g in range(4):
            pool_mm(g)
        dump("cat", cat, CAT_ALL)
        for t in range(4):
            wout_tile(ci, t, [rslot(4), rslot(5)])
        ring_load(gp0 + 4 + RING)
        ring_load(gp0 + 5 + RING)
        for t in range(4):
            dump(f"x1_{t}", xs[(ci * 4 + t) % XSLOTS][:], [("x", (ci * 4 + t) % XSLOTS)])
        if stop <= 5:
            raise _Stop()
        norm_prep(ci)
        norm_tp(b, A2, 24)
        for m in range(32):
            wup_chunk(m, rslot(6 + m // 4))
            if m % 4 == 3:
                ring_load(gp0 + 6 + m // 4 + RING)
        if ci + 1 < total_chunks:
            norm_prep(ci + 1)
        for hf in range(2):
            for kg in range(4):
                for t in range(4):
                    wdown_group(hf, kg, t, rslot(14 + hf * 4 + kg))
                ring_load(gp0 + 14 + hf * 4 + kg + RING)
        s0 = epiA(ci, 0, [0, 4], 1)
        s1 = epiA(ci, 1, [1, 5], 1)
        if ci + 1 < total_chunks:
            norm_tp((ci + 1) // NCH, A1, 0, tpbanks=[0, 4, 1, 5])
        epiB(s0, True)
        epiB(s1, True)
        s2 = epiA(ci, 2, [2, 6], 1)
        s3 = epiA(ci, 3, [3, 7], 1)
        epiB(s2, True)
        epiB(s3, True)
        bank_state["pref"] = [0, 4, 1, 5]
        bank_state["next"] = 2

    try:
        if stop <= 0:
            raise _Stop()
        for ci in range(total_chunks):
            do_chunk(ci)
    except _Stop:
        pass

    sch.finalize()
    eng_sems = {e: es.enter_context(nc.semaphore("sem_" + e)) for e in ("pe", "act", "dve", "pool")}
    dma_sems = {k: es.enter_context(nc.semaphore("dsem_" + k)) for k in dma_keys}
    final_waits = [(dma_sems[k], 16 * sch.dma_cnt[k]) for k in dma_keys]
    block = es.enter_context(nc.Block())

    @block.sync
    def _(eng):
        sch.emit("sp", eng, eng_sems, dma_sems, final_waits)

    @block.tensor
    def _(eng):
        sch.emit("pe", eng, eng_sems, dma_sems)

    @block.scalar
    def _(eng):
        sch.emit("act", eng, eng_sems, dma_sems)

    @block.vector
    def _(eng):
        sch.emit("dve", eng, eng_sems, dma_sems)

    @block.gpsimd
    def _(eng):
        sch.emit("pool", eng, eng_sems, dma_sems)

    es.close()
    return nc


def _consts():
    cst = np.zeros((128, 6, 128), np.float32)
    cst[:, 0, :] = np.eye(128, dtype=np.float32)
    for m in range(64):
        cst[(m + 32) % 64, 1, m] = 1.0
        cst[64 + m, 1, 64 + m] = 1.0
        cst[64 + (m + 32) % 64, 5, m] = 1.0
    k = np.arange(128)[:, None]
    q = np.arange(128)[None, :]
    cst[:, 2, :] = (k <= q).astype(np.float32)
    cst[:, 3, :] = 1.0
    cst[:, 4, :] = np.where(k <= q, 0.0, NEG).astype(np.float32)
    half = 32
    inv_freq = (1.0 / (10000.0 ** (np.arange(half, dtype=np.float32) * np.float32(2.0 / 64)))).astype(np.float32)
    ang = np.arange(S, dtype=np.float32)[None, :] * inv_freq[:, None]
    cos = np.cos(ang).astype(np.float32)
    sin = np.sin(ang).astype(np.float32)
    cos64 = np.concatenate([cos, cos], 0)
    sin64 = np.concatenate([-sin, sin], 0)
    rope = np.stack([cos64.reshape(64, NCH, T), sin64.reshape(64, NCH, T)], axis=2)
    kinit = np.zeros((128, S), np.float32)
    blk = np.arange(S) // 256
    for j in range(8):
        kinit[64 + j, :] = (blk == j).astype(np.float32)
    invc = np.zeros((128, 4, 16), np.float32)
    for g, w in enumerate((2, 4, 8, 16)):
        invc[:, g, :] = 1.0 / np.minimum(np.arange(16) + 1, w)
    return cst, np.ascontiguousarray(rope), kinit, invc


def _piece(w, r0, c0):
    blk = w[r0:r0 + 1024, c0:c0 + 512].reshape(8, 128, 512).transpose(1, 0, 2)
    return np.ascontiguousarray(blk).reshape(128, 4096)


def _prep_shared(inp):
    f = lambda a: np.asarray(a, dtype=np.float32)
    w_in, w_out, w_up, w_down = f(inp["w_in"])[0], f(inp["w_out"])[0], f(inp["w_up"])[0], f(inp["w_down"])[0]
    pieces = []
    pieces.append(_piece(w_in, 0, 512))
    pieces.append(_piece(w_in, 0, 0))
    pieces.append(_piece(w_in, 0, 1024))
    pieces.append(_piece(w_in, 0, 1536))
    for hf in range(2):
        pieces.append(_piece(w_out, 0, hf * 512))
    for j in range(8):
        pieces.append(_piece(w_up, 0, j * 512))
    for hf in range(2):
        for kg in range(4):
            pieces.append(_piece(w_down, kg * 1024, hf * 512))
    W = np.stack(pieces, 0)
    w_ada = f(inp["w_ada"])[0]
    wada = np.stack([_piece(w_ada, 0, pi * 512) for pi in range(12)], 0)
    bada = np.ascontiguousarray(f(inp["b_ada"])[0].reshape(48, 128).T)
    gvec = np.stack([f(inp[k])[0].reshape(8, 128).T for k in ("g_mix_pre", "g_mix_post", "g_mlp_pre", "g_mlp_post")], axis=1)
    pscale = np.ascontiguousarray(f(inp["pool_scale"])[0].reshape(4, 128).T)
    wpool = np.ascontiguousarray(f(inp["w_pool"])[0].transpose(1, 0, 2))
    cst, rope, kinit, invc = _consts()
    return dict(W=W, wada=wada, bada=bada, gvec=np.ascontiguousarray(gvec), pscale=pscale, wpool=wpool,
                cst=cst, rope=rope, kinit=kinit, invc=invc)


def _in_maps(inp, ncores=NCORES, nseq=NSEQ):
    shared = _prep_shared(inp)
    x = np.asarray(inp["x"], dtype=np.float32)
    c = np.asarray(inp["c"], dtype=np.float32)
    maps = []
    for i in range(ncores):
        xb = np.ascontiguousarray(x[i * NSEQ:i * NSEQ + nseq].reshape(nseq * S, D))
        cb = c[i * NSEQ:(i + 1) * NSEQ]
        cT = np.ascontiguousarray(cb.reshape(NSEQ, 8, 128).transpose(2, 1, 0))
        m = dict(shared)
        m["x"] = xb
        m["cT"] = cT
        maps.append(m)
    return maps


def kernel(**inputs):
    nc = bass.Bass("TRN2", target_bir_lowering=False)
    build(nc)
    maps = _in_maps(inputs)
    res = run_bass_kernel_spmd(nc, maps, core_ids=list(range(NCORES)))
    outs = [np.asarray(r["out"]).reshape(NSEQ, S, D) for r in res.results]
    return np.concatenate(outs, axis=0).astype(np.float32)
```
